# Optimizing a Trainium2 kernel written in Bass

```python
import math
import jax, jax.numpy as jnp
from jax import lax
import numpy as np

D_MODEL = 1024
BATCH = 8
SEQ = 2048
DEPTH = 2

GRID_W = 64
N_GROUPS = 4
W_NA = D_MODEL // N_GROUPS
W_SC = D_MODEL // N_GROUPS
W_CF = D_MODEL // N_GROUPS
W_SSM = D_MODEL // N_GROUPS
D_MIX = W_NA + W_SC + W_CF + W_SSM
IN_COLS = 3 * W_NA + 3 * W_SC + 2 * W_CF + W_SSM
NA_HEAD_DIM = 64
NA_HEADS = W_NA // NA_HEAD_DIM
NA_WIN_H = 8
NA_WIN_W = 16
NA_QBLK_W = 16
NA_KBLK_W = NA_QBLK_W + NA_WIN_W
SC_WIDTH = 3
CF_WIDTH = 31
SSM_GROUP_CH = 16
SSM_GROUPS = W_SSM // SSM_GROUP_CH
SSM_STATE = 64
D_FF_DENSE = ((8 * D_MODEL // 3 + 127) // 128) * 128
N_EXPERTS = 8
TOP_K = 2
D_FF_EXPERT = 7 * D_MODEL // 2
N_DENSE = (DEPTH + 1) // 2
N_MOE = DEPTH // 2
EPS = 1e-6
NEG_INF = -1e30

kernel_name = "hybrid_parallel_group_encoder"


def rms_norm(x, g):
    xf = x.astype(jnp.float32)
    y = xf * lax.rsqrt(jnp.mean(xf * xf, axis=-1, keepdims=True) + EPS)
    return (y * g.astype(jnp.float32)).astype(x.dtype)


def layer_norm(x, g, b):
    xf = x.astype(jnp.float32)
    mu = jnp.mean(xf, axis=-1, keepdims=True)
    xc = xf - mu
    var = jnp.mean(xc * xc, axis=-1, keepdims=True)
    return (xc * lax.rsqrt(var + EPS) * g.astype(jnp.float32) + b.astype(jnp.float32)).astype(x.dtype)


def depthwise_conv(x, w):
    k, c = w.shape
    return lax.conv_general_dilated(
        x, w[:, None, :].astype(x.dtype), window_strides=(1,), padding=[(k // 2, k // 2)],
        dimension_numbers=("NWC", "WIO", "NWC"), feature_group_count=c)


def neighbourhood_attention(q, k, v, rpb):
    bsz, seq_len, n_heads, head_dim = q.shape
    rows = seq_len // GRID_W
    kh = min(NA_WIN_H, rows)
    ncb = GRID_W // NA_QBLK_W
    r = np.arange(rows)
    row_start = np.clip(r - kh // 2, 0, rows - kh)
    key_rows = row_start[:, None] + np.arange(kh)
    j = np.arange(ncb)
    q_cols = j[:, None] * NA_QBLK_W + np.arange(NA_QBLK_W)
    q_col_start = np.clip(q_cols - NA_WIN_W // 2, 0, GRID_W - NA_WIN_W)
    kblk_start = np.clip(j * NA_QBLK_W - NA_WIN_W // 2, 0, GRID_W - NA_KBLK_W)
    key_cols = kblk_start[:, None] + np.arange(NA_KBLK_W)
    key_idx = key_rows[:, None, :, None] * GRID_W + key_cols[None, :, None, :]
    flat = key_idx.reshape(-1)
    kg = jnp.take(k, flat, axis=1).reshape(bsz, rows, ncb, kh, NA_KBLK_W, n_heads, head_dim)
    vg = jnp.take(v, flat, axis=1).reshape(bsz, rows, ncb, kh, NA_KBLK_W, n_heads, head_dim)
    qb = q.reshape(bsz, rows, ncb, NA_QBLK_W, n_heads, head_dim)
    s = jnp.einsum("brjqhd,brjkwhd->bhrjqkw", qb, kg).astype(jnp.float32) * (head_dim ** -0.5)
    dr_idx = key_rows - r[:, None] + NA_WIN_H - 1
    dc = key_cols[:, None, :] - q_cols[:, :, None]
    dc_idx = np.clip(dc + NA_WIN_W - 1, 0, 2 * NA_WIN_W - 2)
    bias = rpb.astype(jnp.float32)[:, dr_idx[:, None, None, :, None], dc_idx[None, :, :, None, :]]
    in_win = (key_cols[:, None, :] >= q_col_start[:, :, None]) & (key_cols[:, None, :] < q_col_start[:, :, None] + NA_WIN_W)
    s = jnp.where(in_win[None, None, None, :, :, None, :], s + bias[None], NEG_INF)
    p = jax.nn.softmax(s.reshape(bsz, n_heads, rows, ncb, NA_QBLK_W, kh * NA_KBLK_W), axis=-1)
    p = p.reshape(bsz, n_heads, rows, ncb, NA_QBLK_W, kh, NA_KBLK_W).astype(v.dtype)
    o = jnp.einsum("bhrjqkw,brjkwhd->brjqhd", p, vg)
    return o.reshape(bsz, seq_len, n_heads * head_dim)


def _linear_recurrence(e1, e2):
    a1, b1 = e1
    a2, b2 = e2
    return a2 * a1, a2 * b1 + b2


def s5_mixer(u, a_re, a_im, log_dt, b_re, b_im, c_re, c_im, d_skip, w_glu, b_glu):
    bsz, seq_len, _ = u.shape
    f32 = jnp.float32
    uf = u.astype(f32).reshape(bsz, seq_len, SSM_GROUPS, SSM_GROUP_CH)
    uc = uf.astype(jnp.complex64)
    a = lax.complex(a_re.astype(f32), a_im.astype(f32))
    dt = jnp.exp(log_dt.astype(f32))[..., None]
    a_bar = jnp.exp(a * dt)
    b_bar = ((a_bar - 1.0) / a)[..., None] * lax.complex(b_re.astype(f32), b_im.astype(f32))
    c = lax.complex(c_re.astype(f32), c_im.astype(f32))
    y = d_skip.astype(f32).reshape(SSM_GROUPS, SSM_GROUP_CH) * uf
    for direction in range(2):
        bu = jnp.einsum("gps,blgs->blgp", b_bar[direction], uc)
        decay = jnp.broadcast_to(a_bar[direction], bu.shape)
        _, state = lax.associative_scan(_linear_recurrence, (decay, bu), axis=1, reverse=(direction == 1))
        y = y + jnp.einsum("gsp,blgp->blgs", c[direction], state).real
    y = jax.nn.gelu(y.reshape(bsz, seq_len, W_SSM)).astype(u.dtype)
    return y * jax.nn.sigmoid(y @ w_glu + b_glu)


def token_mixer(h, w_in, na_rpb, sc_conv_w, cf_conv_w, cf_conv_b, cf_ln_g, cf_ln_b,
                ssm_a_re, ssm_a_im, ssm_log_dt, ssm_b_re, ssm_b_im, ssm_c_re, ssm_c_im,
                ssm_d, ssm_w_glu, ssm_b_glu, grp_norm_g, w_out):
    bsz, seq_len, _ = h.shape
    z = h @ w_in
    cuts = [3 * W_NA, 3 * W_NA + 3 * W_SC, 3 * W_NA + 3 * W_SC + 2 * W_CF]
    z_na, z_sc, z_cf, z_ssm = jnp.split(z, cuts, axis=-1)
    qkv = z_na.reshape(bsz, seq_len, 3, NA_HEADS, NA_HEAD_DIM)
    y_na = neighbourhood_attention(qkv[:, :, 0], qkv[:, :, 1], qkv[:, :, 2], na_rpb)
    sc_b, sc_c, sc_x = jnp.split(z_sc, 3, axis=-1)
    y_sc = sc_b * depthwise_conv(sc_c * sc_x, sc_conv_w)
    cf_a, cf_g = jnp.split(z_cf, 2, axis=-1)
    cf = depthwise_conv(cf_a * jax.nn.sigmoid(cf_g), cf_conv_w) + cf_conv_b
    y_cf = jax.nn.silu(layer_norm(cf, cf_ln_g, cf_ln_b))
    y_ssm = s5_mixer(z_ssm, ssm_a_re, ssm_a_im, ssm_log_dt, ssm_b_re, ssm_b_im,
                     ssm_c_re, ssm_c_im, ssm_d, ssm_w_glu, ssm_b_glu)
    y = jnp.concatenate([y_na, y_sc, y_cf, y_ssm], axis=-1).reshape(bsz, seq_len, N_GROUPS, D_MIX // N_GROUPS)
    y = rms_norm(y, grp_norm_g.reshape(N_GROUPS, D_MIX // N_GROUPS)).reshape(bsz, seq_len, D_MIX)
    return y @ w_out


def swiglu(x, w_gate, w_up, w_down):
    return (jax.nn.silu(x @ w_gate) * (x @ w_up)) @ w_down


def moe_swiglu(x, w_router, w_gate, w_up, w_down):
    bsz, seq_len, d = x.shape
    xt = x.reshape(-1, d)
    logits = (xt @ w_router).astype(jnp.float32)
    top_v, top_i = lax.top_k(logits, TOP_K)
    gates = jax.nn.softmax(top_v, axis=-1)
    combine = jnp.sum(jax.nn.one_hot(top_i, N_EXPERTS, dtype=jnp.float32) * gates[..., None], axis=1)
    combine = combine.astype(x.dtype)
    y = jnp.zeros_like(xt)
    for e in range(N_EXPERTS):
        y = y + combine[:, e:e + 1] * swiglu(xt, w_gate[e], w_up[e], w_down[e])
    return y.reshape(bsz, seq_len, d)


def setup_inputs(seed: int = 0) -> dict:
    key = jax.random.key(seed)
    ks = iter(jax.random.split(key, 48))
    f32 = jnp.float32

    def nrm(shape, scale):
        return jax.random.normal(next(ks), shape, f32) * scale

    n_idx = jnp.arange(SSM_STATE, dtype=f32)
    inputs = {
        "x": nrm((BATCH, SEQ, D_MODEL), 1.0),
        "norm1_g": 1.0 + nrm((DEPTH, D_MODEL), 0.05),
        "w_in": nrm((DEPTH, D_MODEL, IN_COLS), D_MODEL ** -0.5),
        "na_rpb": nrm((DEPTH, NA_HEADS, 2 * NA_WIN_H - 1, 2 * NA_WIN_W - 1), 0.1),
        "sc_conv_w": nrm((DEPTH, SC_WIDTH, W_SC), SC_WIDTH ** -0.5),
        "cf_conv_w": nrm((DEPTH, CF_WIDTH, W_CF), CF_WIDTH ** -0.5),
        "cf_conv_b": nrm((DEPTH, W_CF), 0.01),
        "cf_ln_g": 1.0 + nrm((DEPTH, W_CF), 0.05),
        "cf_ln_b": nrm((DEPTH, W_CF), 0.01),
        "ssm_a_re": -0.5 + nrm((DEPTH, 2, SSM_GROUPS, SSM_STATE), 0.01),
        "ssm_a_im": math.pi * n_idx + nrm((DEPTH, 2, SSM_GROUPS, SSM_STATE), 0.01),
        "ssm_log_dt": jax.random.uniform(next(ks), (DEPTH, 2, SSM_GROUPS), f32, math.log(1e-3), math.log(1e-1)),
        "ssm_b_re": nrm((DEPTH, 2, SSM_GROUPS, SSM_STATE, SSM_GROUP_CH), (2 * SSM_GROUP_CH) ** -0.5),
        "ssm_b_im": nrm((DEPTH, 2, SSM_GROUPS, SSM_STATE, SSM_GROUP_CH), (2 * SSM_GROUP_CH) ** -0.5),
        "ssm_c_re": nrm((DEPTH, 2, SSM_GROUPS, SSM_GROUP_CH, SSM_STATE), SSM_STATE ** -0.5),
        "ssm_c_im": nrm((DEPTH, 2, SSM_GROUPS, SSM_GROUP_CH, SSM_STATE), SSM_STATE ** -0.5),
        "ssm_d": nrm((DEPTH, W_SSM), 0.5),
        "ssm_w_glu": nrm((DEPTH, W_SSM, W_SSM), W_SSM ** -0.5),
        "ssm_b_glu": nrm((DEPTH, W_SSM), 0.01),
        "grp_norm_g": 1.0 + nrm((DEPTH, D_MIX), 0.05),
        "w_out": nrm((DEPTH, D_MIX, D_MODEL), D_MIX ** -0.5),
        "norm2_g": 1.0 + nrm((DEPTH, D_MODEL), 0.05),
        "ffn_w_gate": nrm((N_DENSE, D_MODEL, D_FF_DENSE), D_MODEL ** -0.5),
        "ffn_w_up": nrm((N_DENSE, D_MODEL, D_FF_DENSE), D_MODEL ** -0.5),
        "ffn_w_down": nrm((N_DENSE, D_FF_DENSE, D_MODEL), D_FF_DENSE ** -0.5),
        "moe_w_router": nrm((N_MOE, D_MODEL, N_EXPERTS), D_MODEL ** -0.5),
        "moe_w_gate": nrm((N_MOE, N_EXPERTS, D_MODEL, D_FF_EXPERT), D_MODEL ** -0.5),
        "moe_w_up": nrm((N_MOE, N_EXPERTS, D_MODEL, D_FF_EXPERT), D_MODEL ** -0.5),
        "moe_w_down": nrm((N_MOE, N_EXPERTS, D_FF_EXPERT, D_MODEL), D_FF_EXPERT ** -0.5),
        "final_norm_g": 1.0 + nrm((D_MODEL,), 0.05),
    }
    return inputs


def reference(x, norm1_g, w_in, na_rpb, sc_conv_w, cf_conv_w, cf_conv_b, cf_ln_g, cf_ln_b,
              ssm_a_re, ssm_a_im, ssm_log_dt, ssm_b_re, ssm_b_im, ssm_c_re, ssm_c_im,
              ssm_d, ssm_w_glu, ssm_b_glu, grp_norm_g, w_out, norm2_g,
              ffn_w_gate, ffn_w_up, ffn_w_down, moe_w_router, moe_w_gate, moe_w_up, moe_w_down,
              final_norm_g):
    for i in range(DEPTH):
        h = rms_norm(x, norm1_g[i])
        x = x + token_mixer(h, w_in[i], na_rpb[i], sc_conv_w[i], cf_conv_w[i], cf_conv_b[i],
                            cf_ln_g[i], cf_ln_b[i], ssm_a_re[i], ssm_a_im[i], ssm_log_dt[i],
                            ssm_b_re[i], ssm_b_im[i], ssm_c_re[i], ssm_c_im[i], ssm_d[i],
                            ssm_w_glu[i], ssm_b_glu[i], grp_norm_g[i], w_out[i])
        h = rms_norm(x, norm2_g[i])
        if i % 2 == 0:
            j = i // 2
            x = x + swiglu(h, ffn_w_gate[j], ffn_w_up[j], ffn_w_down[j])
        else:
            j = i // 2
            x = x + moe_swiglu(h, moe_w_router[j], moe_w_gate[j], moe_w_up[j], moe_w_down[j])
    return rms_norm(x, final_norm_g)
```

```python
import math
import contextlib
import numpy as np
import concourse.bass as bass
import concourse.mybir as mybir
from concourse.bass_utils import run_bass_kernel_spmd

F32 = mybir.dt.float32
BF16 = mybir.dt.bfloat16
I32 = mybir.dt.int32
ALU = mybir.AluOpType
AF = mybir.ActivationFunctionType
AX = mybir.AxisListType

_DT_SIZE = {}


def dt_size(dt):
    s = str(dt)
    if "64" in s:
        return 8
    if "32" in s:
        return 4
    if "16" in s:
        return 2
    return 1


class Sync:
    EPOCH = 24000
    DMA_RING = 6

    def __init__(self, nc, stack):
        self.nc = nc
        self.stack = stack
        self.eng = dict(pe=nc.tensor, act=nc.scalar, dve=nc.vector, pool=nc.gpsimd, sp=nc.sync)
        self.sem = {}
        self.cnt = {}
        self.pend = {}
        self.nsem = 0
        self.waited = {e: {} for e in self.eng}
        self.wr = {}
        self.rd = {}
        self.semobj = {}
        for e in ("pe", "act", "dve", "pool"):
            self._new_sem(e)
        self.ring = {}
        self.ring_pos = {}
        for q in ("sp", "pool", "act"):
            self.ring[q] = []
            self.ring_pos[q] = 0
        self.n_inst = 0
        self.n_wait = 0

    def _alloc_sem(self, name):
        s = self.stack.enter_context(self.nc.semaphore(f"{name}_{self.nsem}"))
        self.nsem += 1
        self.semobj[id(s)] = s
        return s

    def _new_sem(self, e):
        self.sem[e] = self._alloc_sem("s_" + e)
        self.cnt[e] = 0
        self.pend[e] = False

    @staticmethod
    def region(ap):
        t = ap.tensor
        dims = ap.ap
        pstep, pcount = dims[0]
        off = int(ap.offset)
        if pstep > 0:
            p0 = off // pstep
            foff = off - p0 * pstep
        else:
            p0 = 0
            foff = off
        lo = hi = foff
        for st, c in dims[1:]:
            if c <= 0:
                continue
            d = st * (c - 1)
            if d < 0:
                lo += d
            else:
                hi += d
        sz = dt_size(ap.dtype)
        return (t.name, p0, p0 + pcount, lo * sz, (hi + 1) * sz)

    @staticmethod
    def _ov(a, r):
        return a[0] < r[2] and r[1] < a[1] and a[2] < r[4] and r[3] < a[3]

    @staticmethod
    def _contained(a, r):
        return a[0] >= r[1] and a[1] <= r[2] and a[2] >= r[3] and a[3] <= r[4]

    def _need(self, e, sem, val, deps):
        k = id(sem)
        if self.waited[e].get(k, 0) >= val:
            return
        if deps.get(k, (None, 0))[1] < val:
            deps[k] = (sem, val)

    def _collect(self, e, reads, writes, skip_sem=None):
        deps = {}
        for ap in reads:
            r = self.region(ap)
            for a in self.wr.get(r[0], ()):
                if self._ov(a, r) and a[4] is not skip_sem:
                    self._need(e, a[4], a[5], deps)
        for ap in writes:
            r = self.region(ap)
            for a in self.wr.get(r[0], ()):
                if self._ov(a, r) and a[4] is not skip_sem:
                    self._need(e, a[4], a[5], deps)
            for a in self.rd.get(r[0], ()):
                if self._ov(a, r) and a[4] is not skip_sem:
                    self._need(e, a[4], a[5], deps)
        return deps

    def _emit_waits(self, e, deps):
        for k, (sem, val) in deps.items():
            for en, s in self.sem.items():
                if s is sem and val > self.cnt[en]:
                    raise RuntimeError(f"wait on unsignaled work of {en}: {val} > {self.cnt[en]}")
            self.eng[e].wait_ge(sem, val)
            self.waited[e][k] = val
            self.n_wait += 1

    def _record(self, reads, writes, sem, val):
        for ap in reads:
            r = self.region(ap)
            lst = self.rd.setdefault(r[0], [])
            lst[:] = [a for a in lst if not (a[4] is sem and self._contained(a, r))]
            lst.append((r[1], r[2], r[3], r[4], sem, val))
        for ap in writes:
            r = self.region(ap)
            lst = self.wr.setdefault(r[0], [])
            lst[:] = [a for a in lst if not self._contained(a, r)]
            lst.append((r[1], r[2], r[3], r[4], sem, val))
            lst2 = self.rd.get(r[0])
            if lst2:
                lst2[:] = [a for a in lst2 if not self._contained(a, r)]

    def op(self, e, fn, reads=(), writes=(), signal=True):
        reads = [a for a in reads if a is not None and not isinstance(a, (int, float))]
        skip = self.sem[e] if e == "pe" else None
        deps = self._collect(e, reads, writes, skip_sem=skip)
        self._emit_waits(e, deps)
        ins = fn()
        self.n_inst += 1
        if signal:
            if self.cnt[e] >= self.EPOCH and not self.pend[e]:
                old = self.sem[e]
                oldc = self.cnt[e]
                self._new_sem(e)
            self.cnt[e] += 1
            ins.then_inc(self.sem[e], 1)
            self.pend[e] = False
            self._record(reads, writes, self.sem[e], self.cnt[e])
        else:
            self.pend[e] = True
            self._record(reads, writes, self.sem[e], self.cnt[e] + 1)
        return ins

    def dma(self, q, out, in_, **kw):
        reads = [in_] if str(in_.space) != "DRAM" and "DRAM" not in str(in_.space).upper() else []
        writes = [out] if "DRAM" not in str(out.space).upper() else []
        ring = self.ring[q]
        pos = self.ring_pos[q]
        if len(ring) < self.DMA_RING:
            ring.append([self._alloc_sem("d_" + q), 0])
        slot = ring[pos % self.DMA_RING]
        self.ring_pos[q] = pos + 1
        deps = self._collect(q, reads, writes)
        if slot[1] > 0:
            self._need(q, slot[0], slot[1], deps)
        if slot[1] >= self.EPOCH * 2:
            self._emit_waits(q, deps)
            deps = {}
            slot[0] = self._alloc_sem("d_" + q)
            slot[1] = 0
        self._emit_waits(q, deps)
        ins = self.eng[q].dma_start(out=out, in_=in_, **kw)
        slot[1] += 16
        ins.then_inc(slot[0], 16)
        self.n_inst += 1
        self._record(reads, writes, slot[0], slot[1])
        return ins

    def wait_all_dma(self, e="sp"):
        for q, ring in self.ring.items():
            for sem, val in ring:
                if val > 0 and self.waited[e].get(id(sem), 0) < val:
                    self.eng[e].wait_ge(sem, val)
                    self.waited[e][id(sem)] = val


D = 1024
L = 2048
NCORES = 8
GRID_W = 64
ROWS = 32
NEG = -30000.0
FF_D = 2816
FF_E = 3584
NE = 8
EPS = 1e-6

PV_N1 = 0
PV_N2 = 8
PV_GN = 16
PV_SCW = 24
PV_CFW = 30
PV_CFB = 92
PV_LNG = 94
PV_LNB = 96
PV_BGLU = 98
PV_COLS = 100

C_ONES = 0
C_ID = 128
C_MF = 256
C_MB = 384
C_KK = 512
C_CIDX = 545
C_COLS = 801


def _row_start(r):
    return int(np.clip(r - 4, 0, ROWS - 8))


def na_plan():
    plans = []
    keys = {}
    for i in range(16):
        r0 = 2 * i
        lo = min(_row_start(r0), _row_start(r0 + 1))
        hi = max(_row_start(r0), _row_start(r0 + 1)) + 7
        a0, a1 = lo // 2, hi // 2
        lst = []
        for a in range(a0, a1 + 1):
            key = (2 * a - r0, _row_start(r0) - r0, _row_start(r0 + 1) - r0)
            if key not in keys:
                keys[key] = len(keys)
            lst.append((a, keys[key]))
        plans.append(lst)
    return plans, keys


def na_tables(rpb):
    plans, keys = na_plan()
    out = np.empty((len(keys), 128, 4, 128), np.float32)
    kl = np.arange(128)
    kr_l, kc = kl // 64, kl % 64
    ql = np.arange(128)
    qr_l, qc = ql // 64, ql % 64
    qcs = np.clip(qc - 8, 0, GRID_W - 16)
    for (da, rs0, rs1), idx in keys.items():
        krow = (da + kr_l)[:, None]
        qrow = qr_l[None, :]
        rs = np.where(qr_l == 0, rs0, rs1)[None, :]
        row_ok = (krow >= rs) & (krow < rs + 8)
        col_ok = (kc[:, None] >= qcs[None, :]) & (kc[:, None] < qcs[None, :] + 16)
        dr = np.clip(krow - qrow + 7, 0, 14)
        dc = np.clip(kc[:, None] - qc[None, :] + 15, 0, 30)
        ok = row_ok & col_ok
        for h in range(4):
            out[idx, :, h, :] = np.where(ok, rpb[h][dr, dc], np.float32(NEG))
    return out.reshape(len(keys), 128, 512)


def fm_cols(v, nchunk):
    return np.ascontiguousarray(np.asarray(v, np.float32).reshape(nchunk, 128).T)


def make_consts():
    c = np.zeros((128, C_COLS), np.float32)
    c[:, C_ONES:C_ONES + 128] = 1.0
    c[:, C_ID:C_ID + 128] = np.eye(128, dtype=np.float32)
    sp = np.arange(128)[:, None] // 16
    s = np.arange(128)[None, :] // 16
    c[:, C_MF:C_MF + 128] = (sp <= s)
    c[:, C_MB:C_MB + 128] = (sp >= s)
    kk = np.zeros((128, 33), np.float32)
    sv = np.arange(8)
    kk[:64, 0:8] = -sv
    kk[64:, 0:8] = sv
    kk[:64, 8:16] = 7 - sv
    kk[64:, 8:16] = sv
    kk[:64, 16:24] = sv
    kk[64:, 16:24] = -sv
    kk[:64, 24:32] = sv + 1
    kk[64:, 24:32] = 8 - sv
    kk[:, 32] = 1
    c[:, C_KK:C_KK + 33] = kk
    c[:, C_CIDX:C_CIDX + 256] = np.arange(256)[None, :]
    return c


def host_prep(inp):
    f = lambda a: np.ascontiguousarray(np.asarray(a, np.float32))
    sh = {}
    pv = np.zeros((2, 128, PV_COLS), np.float32)
    for l in range(2):
        pv[l, :, PV_N1:PV_N1 + 8] = fm_cols(inp["norm1_g"][l], 8)
        pv[l, :, PV_N2:PV_N2 + 8] = fm_cols(inp["norm2_g"][l], 8)
        pv[l, :, PV_GN:PV_GN + 8] = fm_cols(inp["grp_norm_g"][l], 8)
        for t in range(3):
            pv[l, :, PV_SCW + 2 * t:PV_SCW + 2 * t + 2] = fm_cols(inp["sc_conv_w"][l][t], 2)
        for t in range(31):
            pv[l, :, PV_CFW + 2 * t:PV_CFW + 2 * t + 2] = fm_cols(inp["cf_conv_w"][l][t], 2)
        pv[l, :, PV_CFB:PV_CFB + 2] = fm_cols(inp["cf_conv_b"][l], 2)
        pv[l, :, PV_LNG:PV_LNG + 2] = fm_cols(inp["cf_ln_g"][l], 2)
        pv[l, :, PV_LNB:PV_LNB + 2] = fm_cols(inp["cf_ln_b"][l], 2)
        pv[l, :, PV_BGLU:PV_BGLU + 2] = fm_cols(inp["ssm_b_glu"][l], 2)
    sh["pv"] = pv
    sh["consts"] = make_consts()
    sh["natab"] = np.stack([na_tables(np.asarray(inp["na_rpb"][l], np.float32)) for l in range(2)])
    a_re = f(inp["ssm_a_re"])
    a_im = f(inp["ssm_a_im"])
    ldt = f(inp["ssm_log_dt"])
    s5a = np.zeros((2, 128, 16, 3), np.float32)
    s5a[..., 0] = a_re.transpose(0, 1, 3, 2).reshape(2, 128, 16)
    s5a[..., 1] = a_im.transpose(0, 1, 3, 2).reshape(2, 128, 16)
    s5a[..., 2] = np.broadcast_to(ldt[:, :, None, :], (2, 2, 64, 16)).reshape(2, 128, 16)
    sh["s5a"] = s5a
    b_re = f(inp["ssm_b_re"])
    b_im = f(inp["ssm_b_im"])
    sh["s5b"] = np.ascontiguousarray(np.stack([b_re, b_im], 0).transpose(1, 2, 4, 3, 0, 5).reshape(2, 128, 16, 2, 16))
    c_re = f(inp["ssm_c_re"])
    c_im = f(inp["ssm_c_im"])
    sh["s5c"] = np.ascontiguousarray(np.stack([c_re, c_im], 0).transpose(1, 2, 5, 3, 0, 4).reshape(2, 128, 16, 2, 16))
    d = f(inp["ssm_d"])
    dd = d.reshape(2, 16, 16)
    sh["s5d"] = np.ascontiguousarray(np.broadcast_to(dd.transpose(0, 2, 1)[:, None, :, :], (2, 8, 16, 16)).reshape(2, 128, 16))
    sh["wglu"] = f(inp["ssm_w_glu"])
    sh["win"] = f(inp["w_in"])
    sh["wout"] = f(inp["w_out"])
    sh["fg"] = f(inp["ffn_w_gate"][0])
    sh["fu"] = f(inp["ffn_w_up"][0])
    sh["fd"] = f(inp["ffn_w_down"][0])
    sh["mg"] = f(inp["moe_w_gate"][0])
    sh["mu"] = f(inp["moe_w_up"][0])
    sh["md"] = f(inp["moe_w_down"][0])
    sh["wr"] = np.ascontiguousarray(f(inp["moe_w_router"][0]).reshape(8, 128, 8).transpose(1, 0, 2))
    sh["gfin"] = np.ascontiguousarray(np.broadcast_to(f(inp["final_norm_g"])[None, :], (128, 1024)))
    x = f(inp["x"])
    per_core = [np.ascontiguousarray(x[b].T) for b in range(NCORES)]
    return sh, per_core


ARENA_BYTES = 208000
O_XT = 0
O_HT = 65536
O_YT = 98304
O_SCR = 131072
O_CONST = 196000
SCR_END = O_CONST
STOP = [99]


def build_program(nlayers=2, dbg=False):
    nc = bass.Bass("TRN2", target_bir_lowering=False)
    dr = {}

    def din(name, shape):
        dr[name] = nc.dram_tensor(name, list(shape), F32, kind="ExternalInput").ap()
        return dr[name]

    xT_d = din("xT", [D, L])
    win_d = din("win", [2, D, 2304])
    wout_d = din("wout", [2, D, D])
    fg_d = din("fg", [D, FF_D])
    fu_d = din("fu", [D, FF_D])
    fd_d = din("fd", [FF_D, D])
    mg_d = din("mg", [NE, D, FF_E])
    mu_d = din("mu", [NE, D, FF_E])
    md_d = din("md", [NE, FF_E, D])
    wr_d = din("wr", [128, 8, 8])
    pv_d = din("pv", [2, 128, PV_COLS])
    consts_d = din("consts", [128, C_COLS])
    plans, tkeys = na_plan()
    NT = len(tkeys)
    natab_d = din("natab", [2, NT, 128, 512])
    s5a_d = din("s5a", [2, 128, 16, 3])
    s5b_d = din("s5b", [2, 128, 16, 2, 16])
    s5c_d = din("s5c", [2, 128, 16, 2, 16])
    s5d_d = din("s5d", [2, 128, 16])
    wglu_d = din("wglu", [2, 256, 256])
    gfin_d = din("gfin", [128, 1024])
    out_d = nc.dram_tensor("out", [L, D], F32, kind="ExternalOutput").ap()
    dbg_d = None
    if dbg:
        dbg_d = nc.dram_tensor("dbg", [8, 128, 8, 2048], F32, kind="ExternalOutput").ap()

    st = contextlib.ExitStack()
    with st:
        S = Sync(nc, st)
        arena_t = st.enter_context(nc.sbuf_tensor("arena", [128, ARENA_BYTES // 4], F32))
        arena = arena_t[:]
        ps = [st.enter_context(nc.psum_tensor(f"ps{i}", [128, 512], F32))[:] for i in range(8)]
        EN = dict(dve=nc.vector, pool=nc.gpsimd)

        def V(off, shape, dt=F32):
            assert off % 4 == 0
            n = 1
            for s_ in shape[1:]:
                n *= s_
            nb = n * dt_size(dt)
            assert nb % 4 == 0
            ap = arena[:, off // 4: off // 4 + nb // 4]
            if dt != F32:
                ap = ap.bitcast(dt)
            if len(shape) == 3:
                ap = ap.rearrange("p (a b) -> p a b", a=shape[1])
            elif len(shape) == 4:
                ap = ap.rearrange("p (a b c) -> p a b c", a=shape[1], b=shape[2])
            elif len(shape) == 5:
                ap = ap.rearrange("p (a b c d) -> p a b c d", a=shape[1], b=shape[2], c=shape[3])
            return ap

        def mm(out, lhsT, rhs, start=True, stop=True, sig=None, **kw):
            if lhsT.partition_size() <= 64 and "tile_position" not in kw:
                kw["tile_position"] = (lhsT.base_partition(), 0)
            S.op("pe", lambda: nc.tensor.matmul(out, lhsT, rhs, start=start, stop=stop, **kw),
                 reads=[lhsT, rhs], writes=[out], signal=(stop if sig is None else sig))

        def tr(out, in_, ident):
            S.op("pe", lambda: nc.tensor.transpose(out, in_, ident), reads=[in_, ident], writes=[out])

        def act(out, in_, func, bias=None, scale=None, accum=None):
            kw = {}
            if bias is not None:
                kw["bias"] = bias
            if scale is not None:
                kw["scale"] = scale
            if accum is not None:
                kw["accum_out"] = accum
            S.op("act", lambda: nc.scalar.activation(out, in_, func, **kw),
                 reads=[in_, bias, scale], writes=[out] + ([accum] if accum is not None else []))

        def tt(e, out, a, b, op):
            S.op(e, lambda: EN[e].tensor_tensor(out, a, b, op), reads=[a, b], writes=[out])

        def ts(e, out, a, s1, s2, op0, op1=None):
            if op1 is None:
                S.op(e, lambda: EN[e].tensor_scalar(out, a, s1, None, op0), reads=[a, s1], writes=[out])
            else:
                S.op(e, lambda: EN[e].tensor_scalar(out, a, s1, s2, op0, op1), reads=[a, s1, s2], writes=[out])

        def stt(e, out, a, s, b, op0, op1):
            e = "dve"
            S.op(e, lambda: EN[e].scalar_tensor_tensor(out, a, s, b, op0, op1), reads=[a, s, b], writes=[out])

        def cp(e, out, a):
            if e == "act":
                S.op("act", lambda: nc.scalar.copy(out, a), reads=[a], writes=[out])
            else:
                S.op(e, lambda: EN[e].tensor_copy(out, a), reads=[a], writes=[out])

        def memset(e, out, val):
            S.op(e, lambda: EN[e].memset(out, val), reads=[], writes=[out])

        def recip(out, a):
            S.op("dve", lambda: nc.vector.reciprocal(out, a), reads=[a], writes=[out])

        dbg_slot = [0]

        def dump(ap_fm, nchunk, cast=False):
            if not dbg:
                return
            i = dbg_slot[0]
            dbg_slot[0] += 1
            for k in range(nchunk):
                S.dma("pool", dbg_d[i, :, k, :], ap_fm[:, k, :])

        xT = V(O_XT, [128, 8, L])
        hT = V(O_HT, [128, 8, L], BF16)
        yT = V(O_YT, [128, 8, L], BF16)
        consts = V(O_CONST, [128, C_COLS])
        pvs = V(O_CONST + 3204, [128, 2, PV_COLS])
        o_misc = O_CONST + 3204 + 800
        ident_bf = V(o_misc, [128, 128], BF16)
        ones_bf = V(o_misc + 256, [128, 128], BF16)
        small = V(o_misc + 512, [128, 64])
        assert o_misc + 768 <= ARENA_BYTES
        ones_f = consts[:, C_ONES:C_ONES + 128]
        ident_f = consts[:, C_ID:C_ID + 128]

        S.dma("sp", consts, consts_d)
        for l in range(2):
            S.dma("sp", pvs[:, l, :], pv_d[l])
        xTv = xT_d.rearrange("(k p) t -> p k t", p=128)
        for k in range(8):
            S.dma("sp", xT[:, k, :], xTv[:, k, :])
        cp("dve", ident_bf, ident_f)
        cp("dve", ones_bf, ones_f)

        def rstd_block(chunks, inv_n, rstd_out, bank, sqa, sqb):
            nk = len(chunks)
            for k, c in enumerate(chunks):
                sq = sqa if k % 2 == 0 else sqb
                act(sq, c, AF.Square)
                mm(bank, ones_f, sq, start=(k == 0), stop=(k == nk - 1), sig=True)
            act(rstd_out, bank, AF.Sqrt, bias=eps_col, scale=inv_n)
            recip(rstd_out, rstd_out)

        eps_col = small[:, 0:1]
        memset("dve", eps_col, EPS)

        def norm_fm(gcol0, l, o_tmp):
            sqa = V(o_tmp, [128, 512])
            sqb = V(o_tmp + 2048, [128, 512])
            for tb in range(4):
                rs = V(o_tmp + 4096 + (tb % 2) * 2048, [128, 512])
                sl = slice(tb * 512, (tb + 1) * 512)
                rstd_block([xT[:, k, sl] for k in range(8)], 1.0 / D, rs, ps[7], sqa, sqb)
                for k in range(8):
                    stt("dve" if k % 2 == 0 else "pool", hT[:, k, sl], xT[:, k, sl],
                        pvs[:, l, gcol0 + k:gcol0 + k + 1], rs, ALU.mult, ALU.mult)

        def gnorm_fm(src, gi, l, o_tmp):
            sqa = V(o_tmp, [128, 512])
            sqb = V(o_tmp + 2048, [128, 512])
            for tb in range(4):
                rs = V(o_tmp + 4096 + (tb % 2) * 2048, [128, 512])
                sl = slice(tb * 512, (tb + 1) * 512)
                rstd_block([src[:, c, sl] for c in range(2)], 1.0 / 256, rs, ps[7], sqa, sqb)
                for c in range(2):
                    gc = PV_GN + 2 * gi + c
                    stt("dve", yT[:, 2 * gi + c, sl], src[:, c, sl], pvs[:, l, gc:gc + 1], rs, ALU.mult, ALU.mult)

        class WRing:
            def __init__(self, off, nbuf, shape):
                n = 1
                for s_ in shape[1:]:
                    n *= s_
                self.bufs = [V(off + i * n * 2, shape, BF16) for i in range(nbuf)]
                self.i = 0
                self.nbytes = nbuf * n * 2

            def load(self, src, sub=None):
                b = self.bufs[self.i % len(self.bufs)]
                self.i += 1
                dst = b if sub is None else sub(b)
                if len(src.shape) == 3 and src.shape[1] > 1:
                    for k in range(src.shape[1]):
                        S.dma("pool", dst[:, k, :], src[:, k, :])
                else:
                    S.dma("pool", dst, src)
                return b

        def proj_fm(wv, col0, ncols, actT, K, consumer, ring, banks, unit=256):
            nunits = ncols // unit
            loaded = {}

            def ld(u):
                loaded[u] = ring.load(wv[:, :, col0 + u * unit: col0 + (u + 1) * unit])
            ld(0)
            if nunits > 1:
                ld(1)
            bi = 0
            for u in range(nunits):
                if u + 2 < nunits:
                    ld(u + 2)
                wb = loaded.pop(u)
                for mi in range(unit // 128):
                    m = u * (unit // 128) + mi
                    for tb in range(4):
                        bank = banks[bi % len(banks)]
                        bi += 1
                        for k in range(K):
                            mm(bank, wb[:, k, mi * 128:(mi + 1) * 128], actT[:, k, tb * 512:(tb + 1) * 512],
                               start=(k == 0), stop=(k == K - 1))
                        consumer(m, tb, bank)


        def mixer_attention(l, winv, ring):
            o = O_SCR + ring.nbytes
            QT = V(o, [128, 2, L], BF16); o += 8192
            KT = V(o, [128, 2, L], BF16); o += 8192
            Vaug = V(o, [128, 16, 4, 65], BF16); o += 8320
            Etab = V(o, [128, NT, 512], BF16); o += NT * 1024
            Pb = [V(o + i * 1024, [128, 512], BF16) for i in range(2)]; o += 2048
            stg = [V(o, [128, 512])] * 2; o += 2048
            ytok = V(o, [128, 4, 64]); o += 1024
            ybf = V(o, [128, 256], BF16); o += 512
            junk = stg[0][:, 0:256]
            assert o <= SCR_END, o
            ss = small[:, 1:2]
            rec4 = small[:, 4:8]

            for t in range(NT):
                S.dma("sp", stg[t % 2], natab_d[l, t])
                act(Etab[:, t, :], stg[t % 2], AF.Exp)
            memset("pool", Vaug[:, :, :, 64:65], 1.0)

            if STOP[0] == 10:
                return
            def consumer(m, tb, bank):
                dst = QT if m < 2 else KT
                cp("act", dst[:, m % 2, tb * 512:(tb + 1) * 512], bank)
            proj_fm(winv, 0, 512, hT, 8, consumer, ring, ps[0:4])
            if STOP[0] == 11:
                return
            wvb = ring.load(winv[:, :, 512:768])
            for t in range(16):
                bank = ps[t % 4]
                for k in range(8):
                    mm(bank[:, 0:256], hT[:, k, t * 128:(t + 1) * 128], wvb[:, k, :], start=(k == 0), stop=(k == 7))
                cp("dve", Vaug[:, t, :, 0:64], bank[:, 0:256].rearrange("p (h d) -> p h d", h=4))
            if STOP[0] == 12:
                return
            ptr_bf = ps[7].bitcast(BF16)
            for i in range(16):
                if STOP[0] == 13 and i == 1:
                    return
                pvb = ps[6]
                pvv = pvb[:, 0:260].rearrange("p (h d) -> p h d", h=4)
                plan = plans[i]
                POS = {0: 0, 2: 1, 1: 2, 3: 3}
                for ci, (a, ti) in enumerate(plan):
                    sb0, sb1 = (ps[4], ps[5]) if ci % 2 == 0 else (ps[2], ps[3])
                    for h in range(4):
                        hp = (h % 2) * 64
                        sbh = sb0 if hp == 0 else sb1
                        mm(sbh[:, (h // 2) * 128:(h // 2 + 1) * 128], KT[hp:hp + 64, h // 2, a * 128:(a + 1) * 128],
                           QT[hp:hp + 64, h // 2, i * 128:(i + 1) * 128], start=True, stop=True, sig=True)
                    P = Pb[ci % 2]
                    Ev = Etab[:, ti, :].rearrange("p (h q) -> p h q", h=4)
                    act(P[:, 0:256], sb0[:, 0:256], AF.Exp, scale=0.125)
                    act(P[:, 256:512], sb1[:, 0:256], AF.Exp, scale=0.125)
                    tt("dve", P[:, 0:256].rearrange("p (h q) -> p h q", h=2), P[:, 0:256].rearrange("p (h q) -> p h q", h=2),
                       Ev[:, 0::2, :], ALU.mult)
                    tt("dve", P[:, 256:512].rearrange("p (h q) -> p h q", h=2), P[:, 256:512].rearrange("p (h q) -> p h q", h=2),
                       Ev[:, 1::2, :], ALU.mult)
                    for h in range(4):
                        first = (ci == 0 and h == 0)
                        last = (ci == len(plan) - 1 and h == 3)
                        mm(pvv[:, h, :], P[:, POS[h] * 128:(POS[h] + 1) * 128], Vaug[:, a, h, :], start=first, stop=last,
                           sig=(h == 3), skip_group_check=True)
                if STOP[0] == 14:
                    return
                recip(rec4, pvv[:, :, 64])
                tt("dve", ytok, pvv[:, :, 0:64], rec4.unsqueeze(2).broadcast_to([128, 4, 64]), ALU.mult)
                yflat = ytok.rearrange("p h d -> p (h d)")
                act(junk, yflat, AF.Square, accum=ss)
                act(ss, ss, AF.Sqrt, bias=eps_col, scale=1.0 / 256)
                recip(ss, ss)
                ts("dve", ybf, yflat, ss, None, ALU.mult)
                for m in range(2):
                    tr(ptr_bf[:, m * 128:(m + 1) * 128], ybf[:, m * 128:(m + 1) * 128], ident_bf)
                for m in range(2):
                    gc = PV_GN + m
                    ts("dve", yT[:, m, i * 128:(i + 1) * 128], ptr_bf[:, m * 128:(m + 1) * 128],
                       pvs[:, l, gc:gc + 1], None, ALU.mult)

        def mixer_sconv(l, winv, ring):
            o = O_SCR + ring.nbytes
            scb = V(o, [128, 2, L], BF16); o += 8192
            scv = V(o, [128, 2, L]); o += 16384
            sco = V(o, [128, 2, L]); o += 16384
            tmp = o
            assert o + 8192 <= SCR_END

            def consumer(m, tb, bank):
                mm_ = m - 6
                sl = slice(tb * 512, (tb + 1) * 512)
                if mm_ < 2:
                    cp("act", scb[:, mm_, sl], bank)
                elif mm_ < 4:
                    cp("act", scv[:, mm_ - 2, sl], bank)
                else:
                    tt("dve", scv[:, mm_ - 4, sl], scv[:, mm_ - 4, sl], bank, ALU.mult)
            proj_fm(winv, 768, 768, hT, 8, lambda m, tb, bank: consumer(m + 6, tb, bank), ring, ps[0:4])
            for c in range(2):
                w = lambda t: pvs[:, l, PV_SCW + 2 * t + c:PV_SCW + 2 * t + c + 1]
                ts("dve", sco[:, c, :], scv[:, c, :], w(1), None, ALU.mult)
                stt("dve", sco[:, c, 1:L], scv[:, c, 0:L - 1], w(0), sco[:, c, 1:L], ALU.mult, ALU.add)
                stt("dve", sco[:, c, 0:L - 1], scv[:, c, 1:L], w(2), sco[:, c, 0:L - 1], ALU.mult, ALU.add)
                tt("pool", sco[:, c, :], sco[:, c, :], scb[:, c, :], ALU.mult)
            gnorm_fm(sco, 1, l, tmp)

        def mixer_conformer(l, winv, ring):
            o = O_SCR + ring.nbytes
            cfa = V(o, [128, 2, L]); o += 16384
            PADW = L + 32
            vpad = V(o, [128, 2, PADW], BF16); o += 2 * PADW * 2
            dg = V(o, [128, 2, 31, 128], BF16); o += 2 * 31 * 256
            sig = [V(o, [128, 512])] * 2; o += 2048
            ycf = cfa
            tmp = o
            assert o + 8192 <= SCR_END, o
            for c in range(2):
                memset("pool", vpad[:, c, 0:15], 0.0)
                memset("pool", vpad[:, c, 15 + L:PADW], 0.0)
                for t in range(31):
                    col = PV_CFW + 2 * t + c
                    ts("pool", dg[:, c, t, :], ident_f, pvs[:, l, col:col + 1], None, ALU.mult)

            def consumer(m, tb, bank):
                sl = slice(tb * 512, (tb + 1) * 512)
                if m < 2:
                    cp("act", cfa[:, m, sl], bank)
                else:
                    sg = sig[tb % 2]
                    act(sg, bank, AF.Sigmoid)
                    tt("dve", vpad[:, m - 2, 15 + tb * 512:15 + (tb + 1) * 512], cfa[:, m - 2, sl], sg, ALU.mult)
            proj_fm(winv, 1536, 512, hT, 8, consumer, ring, ps[0:4])
            bi = 0
            for c in range(2):
                for tb in range(4):
                    bank = ps[bi % 4]; bi += 1
                    for t in range(31):
                        mm(bank, dg[:, c, t, :], vpad[:, c, tb * 512 + t: tb * 512 + t + 512], start=(t == 0), stop=(t == 30))
                    act(cfa[:, c, tb * 512:(tb + 1) * 512], bank, AF.Identity, bias=pvs[:, l, PV_CFB + c:PV_CFB + c + 1])
            sqa = V(tmp, [128, 512]); sqb = V(tmp + 2048, [128, 512])
            mean = V(tmp + 4096, [128, 512]); var = V(tmp + 6144, [128, 512])
            for tb in range(4):
                sl = slice(tb * 512, (tb + 1) * 512)
                for c in range(2):
                    mm(ps[6], ones_f, cfa[:, c, sl], start=(c == 0), stop=(c == 1))
                for c in range(2):
                    sq = sqa if c == 0 else sqb
                    act(sq, cfa[:, c, sl], AF.Square)
                    mm(ps[7], ones_f, sq, start=(c == 0), stop=(c == 1))
                ts("dve", mean, ps[6], 1.0 / 256, None, ALU.mult)
                tt("dve", sqa, mean, mean, ALU.mult)
                stt("dve", var, ps[7], 1.0 / 256, sqa, ALU.mult, ALU.subtract)
                act(var, var, AF.Sqrt, bias=eps_col, scale=1.0)
                recip(var, var)
                for c in range(2):
                    tt("dve", sqb, cfa[:, c, sl], mean, ALU.subtract)
                    tt("dve", sqb, sqb, var, ALU.mult)
                    act(ycf[:, c, sl], sqb, AF.Silu, bias=pvs[:, l, PV_LNB + c:PV_LNB + c + 1],
                        scale=pvs[:, l, PV_LNG + c:PV_LNG + c + 1])
            gnorm_fm(ycf, 2, l, tmp)


        PI = math.pi

        def mixer_s5(l, winv, ring):
            o = O_SCR
            M_sb = V(o, [128, 16, 128], BF16); o += 4096
            WS_sb = V(o, [128, 16, 2, 128], BF16); o += 8192
            WX_sb = V(o, [128, 16, 2, 128], BF16); o += 8192
            r8 = V(o, [128, 16]); o += 64
            th8 = V(o, [128, 16]); o += 64
            o_run = o
            negpi = small[:, 8:9]
            memset("dve", negpi, -PI)

            def sincos(ang, osin, ocos, tmp, tmp2):
                ts("dve", tmp.bitcast(I32), ang, 1.0 / (2 * PI), None, ALU.mult)
                cp("dve", tmp2, tmp.bitcast(I32))
                stt("dve", tmp2, tmp2, -2 * PI, ang, ALU.mult, ALU.add)
                act(tmp, tmp2, AF.Sin, scale=0.5)
                act(ocos, tmp2, AF.Sin, scale=0.25)
                tt("dve", ocos, ocos, ocos, ALU.mult)
                ts("dve", ocos, ocos, -2.0, 1.0, ALU.mult, ALU.add)
                stt("dve", osin, tmp, 2.0, ocos, ALU.mult, ALU.mult)
                tt("dve", ocos, tmp, tmp, ALU.mult)
                ts("dve", ocos, ocos, -2.0, 1.0, ALU.mult, ALU.add)

            sa = V(o, [128, 16, 3]); o += 192
            sbp = V(o, [128, 16, 2, 16]); o += 2048
            scp = V(o, [128, 16, 2, 16]); o += 2048
            sdp = V(o, [128, 16]); o += 64
            S.dma("sp", sa, s5a_d[l])
            S.dma("sp", sbp, s5b_d[l])
            S.dma("sp", scp, s5c_d[l])
            S.dma("sp", sdp, s5d_d[l])
            sm = [V(o + i * 64, [128, 16]) for i in range(10)]; o += 640
            dtv, arv, thv, nr, den, gr, gi, t0, t1, li1 = sm
            a_re = sa[:, :, 0]
            a_im = sa[:, :, 1]
            act(dtv, sa[:, :, 2], AF.Exp)
            tt("dve", arv, a_re, dtv, ALU.mult)
            tt("dve", thv, a_im, dtv, ALU.mult)
            ts("dve", t0, arv, 8.0, None, ALU.mult)
            act(r8, t0, AF.Exp)
            ts("dve", th8, thv, 8.0, None, ALU.mult)
            NS = 33
            ark = V(o, [128, 16, NS]); o += 16 * NS * 4
            thk = V(o, [128, 16, NS]); o += 16 * NS * 4
            LR = V(o, [128, 16, NS]); o += 16 * NS * 4
            LI = V(o, [128, 16, NS]); o += 16 * NS * 4
            tk = V(o, [128, 16, NS]); o += 16 * NS * 4
            tk2 = V(o, [128, 16, NS]); o += 16 * NS * 4
            KKb = consts[:, C_KK:C_KK + NS].unsqueeze(1).broadcast_to([128, 16, NS])
            tt("dve", ark, KKb, arv.unsqueeze(2).broadcast_to([128, 16, NS]), ALU.mult)
            tt("dve", thk, KKb, thv.unsqueeze(2).broadcast_to([128, 16, NS]), ALU.mult)
            act(ark, ark, AF.Exp)
            sincos(thk, LI, LR, tk, tk2)
            tt("dve", LR, LR, ark, ALU.mult)
            tt("dve", LI, LI, ark, ALU.mult)
            ts("dve", nr, LR[:, :, 32], -1.0, None, ALU.add)
            cp("dve", li1, LI[:, :, 32])
            tt("dve", den, a_re, a_re, ALU.mult)
            tt("dve", t0, a_im, a_im, ALU.mult)
            tt("dve", den, den, t0, ALU.add)
            recip(den, den)
            tt("dve", gr, nr, a_re, ALU.mult)
            tt("dve", t0, li1, a_im, ALU.mult)
            tt("dve", gr, gr, t0, ALU.add)
            tt("dve", gr, gr, den, ALU.mult)
            tt("dve", gi, li1, a_re, ALU.mult)
            tt("dve", t0, nr, a_im, ALU.mult)
            tt("dve", gi, gi, t0, ALU.subtract)
            tt("dve", gi, gi, den, ALU.mult)
            bbr = V(o, [128, 16, 16]); o += 1024
            bbi = V(o, [128, 16, 16]); o += 1024
            tb_ = V(o, [128, 16, 16]); o += 1024
            Br = sbp[:, :, 0, :]
            Bi = sbp[:, :, 1, :]
            grb = gr.unsqueeze(2).broadcast_to([128, 16, 16])
            gib = gi.unsqueeze(2).broadcast_to([128, 16, 16])
            tt("dve", bbr, Br, grb, ALU.mult)
            tt("dve", tb_, Bi, gib, ALU.mult)
            tt("dve", bbr, bbr, tb_, ALU.subtract)
            tt("dve", bbi, Bi, grb, ALU.mult)
            tt("dve", tb_, Br, gib, ALU.mult)
            tt("dve", bbi, bbi, tb_, ALU.add)
            Cr = scp[:, :, 0, :]
            Ci = scp[:, :, 1, :]
            big = [V(o + i * 2048, [128, 4, 8, 16]) for i in range(9)]; o += 9 * 2048
            assert o <= SCR_END, o
            Pr, Pi, WSr, WSi, Qr, Qi, WXr, WXi, tmpb = big
            MFb = consts[:, C_MF:C_MF + 128].unsqueeze(1).broadcast_to([128, 4, 128])
            MBb = consts[:, C_MB:C_MB + 128].unsqueeze(1).broadcast_to([128, 4, 128])

            def cmul(outr, outi, slot0, vr, vi, gs, neg_i=False):
                lr = LR[:, gs, slot0:slot0 + 8].unsqueeze(3).broadcast_to([128, 4, 8, 16])
                li = LI[:, gs, slot0:slot0 + 8].unsqueeze(3).broadcast_to([128, 4, 8, 16])
                vrb = vr[:, gs, :].unsqueeze(2).broadcast_to([128, 4, 8, 16])
                vib = vi[:, gs, :].unsqueeze(2).broadcast_to([128, 4, 8, 16])
                tt("dve", outr, lr, vrb, ALU.mult)
                tt("pool", tmpb, li, vib, ALU.mult)
                tt("dve", outr, outr, tmpb, ALU.subtract)
                if not neg_i:
                    tt("dve", outi, lr, vib, ALU.mult)
                    tt("pool", tmpb, li, vrb, ALU.mult)
                    tt("dve", outi, outi, tmpb, ALU.add)
                else:
                    tt("dve", outi, lr, vib, ALU.mult)
                    tt("pool", tmpb, li, vrb, ALU.mult)
                    tt("dve", outi, outi, tmpb, ALU.add)
                    ts("dve", outi, outi, -1.0, None, ALU.mult)

            if STOP[0] == 20:
                return
            for qq in range(4):
                gs = slice(4 * qq, 4 * qq + 4)
                g0 = 4 * qq
                cmul(Pr, Pi, 0, bbr, bbi, gs)
                cmul(WSr, WSi, 8, bbr, bbi, gs)
                cmul(Qr, Qi, 16, Cr, Ci, gs, neg_i=True)
                cmul(WXr, WXi, 24, Cr, Ci, gs, neg_i=True)
                if STOP[0] == 21:
                    return
                f2 = lambda a_: a_.rearrange("p g s j -> p g (s j)")
                Pr2, Pi2, WSr2, WSi2, Qr2, Qi2, WXr2, WXi2 = [f2(a_) for a_ in big[:8]]
                bF, bB = ps[0 + (qq % 2) * 2], ps[1 + (qq % 2) * 2]
                for gi_ in range(4):
                    cs_ = slice(gi_ * 128, (gi_ + 1) * 128)
                    mm(bF[:, cs_], Pr2[0:64, gi_, :], Qr2[0:64, gi_, :], start=True, stop=False, sig=False)
                    mm(bF[:, cs_], Pi2[0:64, gi_, :], Qi2[0:64, gi_, :], start=False, stop=True, sig=(gi_ == 3),
                       skip_group_check=True)
                for gi_ in range(4):
                    cs_ = slice(gi_ * 128, (gi_ + 1) * 128)
                    mm(bB[:, cs_], Pr2[64:128, gi_, :], Qr2[64:128, gi_, :], start=True, stop=False, sig=False)
                    mm(bB[:, cs_], Pi2[64:128, gi_, :], Qi2[64:128, gi_, :], start=False, stop=True, sig=(gi_ == 3),
                       skip_group_check=True)
                tmpM = tmpb.rearrange("p g s j -> p g (s j)")
                tt("dve", tmpM, bF.rearrange("p (g m) -> p g m", g=4), MFb, ALU.mult)
                tt("dve", Pr2, bB.rearrange("p (g m) -> p g m", g=4), MBb, ALU.mult)
                tt("dve", tmpM, tmpM, Pr2, ALU.add)
                for gi_ in range(4):
                    stt("dve", M_sb[:, g0 + gi_, :], ident_f, sdp[:, g0 + gi_:g0 + gi_ + 1], tmpM[:, gi_, :],
                        ALU.mult, ALU.add)
                if STOP[0] == 22:
                    return
                for ri, arr in enumerate((WSr2, WSi2)):
                    bank = ps[4 + ri]
                    for gi_ in range(4):
                        tr(bank[:, gi_ * 128:(gi_ + 1) * 128], arr[:, gi_, :], ident_f)
                    cp("act", WS_sb[:, g0:g0 + 4, ri, :], bank.rearrange("p (g m) -> p g m", g=4))
                cp("act", WX_sb[:, gs, 0, :], WXr2)
                cp("act", WX_sb[:, gs, 1, :], WXi2)

            if STOP[0] == 23:
                return
            o = o_run
            U = V(o, [128, 16, 256], BF16); o += 8192
            Y8 = V(o, [128, 2, 8, 256], BF16); o += 8192
            o_b = o
            wss = V(o, [128, 8, 256], BF16)
            Z8g = V(o + 4096, [128, 2, 16, 8, 16], BF16)
            NB = 2
            cs_t = V(o, [128, NB, 256]); o += 2048
            sn_t = V(o, [128, NB, 256]); o += 2048
            ang = V(o, [128, NB, 256]); o += 2048
            tq = [V(o + i * 2048, [128, NB, 256]) for i in range(6)]; o += 6 * 2048
            Wr, Wi, Wr2, Wi2 = tq[0], tq[1], tq[4], tq[5]
            o = max(o, o_b + 12288)
            Xr = V(o, [128, NB, 258], BF16); o += 1032
            Xi = V(o, [128, NB, 258], BF16); o += 1032
            Yg = V(o, [128, 256], BF16); o += 512
            g1 = V(o, [128, 256]); o += 1024
            assert o <= SCR_END, o
            S.dma("pool", wss, winv[:, :, 2048:2304])
            ptr_bf = ps[7].bitcast(BF16)
            for cb in range(2):
                for s in range(8):
                    bank = ps[(cb * 8 + s) % 4]
                    c0 = 1024 * cb + s
                    for k in range(8):
                        mm(bank[:, 0:256], hT[:, k, c0:c0 + 1017:8], wss[:, k, :], start=(k == 0), stop=(k == 7))
                    cp("act", Z8g[:, cb, :, s, :], bank[:, 0:256].rearrange("p (g j) -> p g j", g=16))
            for g4 in range(4):
                for gi_ in range(4):
                    g = g4 * 4 + gi_
                    for cb in range(2):
                        tr(ptr_bf[:, gi_ * 256 + cb * 128: gi_ * 256 + (cb + 1) * 128],
                           Z8g[:, cb, g, :, :].rearrange("p s j -> p (s j)"), ident_bf)
                cp("dve", U[:, g4 * 4:(g4 + 1) * 4, :], ptr_bf.rearrange("p (g c) -> p g c", g=4))
            if STOP[0] == 24:
                return
            memset("dve", Xr[:, :, 0:1], 0.0)
            memset("dve", Xi[:, :, 0:1], 0.0)
            memset("dve", Xr[:, :, 255:257], 0.0)
            memset("dve", Xi[:, :, 255:257], 0.0)
            cidx = consts[:, C_CIDX:C_CIDX + 256].unsqueeze(1).broadcast_to([128, NB, 256])
            for gb in range(8):
                gsl = slice(gb * NB, gb * NB + NB)
                bSr = ps[0 + 2 * (gb % 2)]
                bSi = ps[1 + 2 * (gb % 2)]
                for gi_ in range(NB):
                    g = gb * NB + gi_
                    csl = slice(gi_ * 256, gi_ * 256 + 256)
                    mm(bSr[:, csl], WS_sb[:, g, 0, :], U[:, g, :], sig=(gi_ == NB - 1))
                for gi_ in range(NB):
                    g = gb * NB + gi_
                    csl = slice(gi_ * 256, gi_ * 256 + 256)
                    mm(bSi[:, csl], WS_sb[:, g, 1, :], U[:, g, :], sig=(gi_ == NB - 1))
                tt("dve", ang, cidx, th8[:, gsl].unsqueeze(2).broadcast_to([128, NB, 256]), ALU.mult)
                sincos(ang, sn_t, cs_t, tq[2], tq[3])
                for d_ in range(2):
                    pp = slice(64 * d_, 64 * d_ + 64)
                    sr = bSr[pp, :].rearrange("p (g c) -> p g c", g=NB)
                    si = bSi[pp, :].rearrange("p (g c) -> p g c", g=NB)
                    if d_ == 1:
                        sr = sr[:, :, ::-1]
                        si = si[:, :, ::-1]
                    c_ = cs_t[pp]
                    s_ = sn_t[pp]
                    a_, b_ = tq[2][pp], tq[3][pp]
                    tt("dve", a_, sr, c_, ALU.mult)
                    tt("dve", b_, si, s_, ALU.mult)
                    tt("pool", Wr[pp], a_, b_, ALU.add)
                    tt("dve", a_, si, c_, ALU.mult)
                    tt("dve", b_, sr, s_, ALU.mult)
                    tt("pool", Wi[pp], a_, b_, ALU.subtract)
                if STOP[0] == 25:
                    return
                for gi_ in range(NB):
                    g = gb * NB + gi_
                    r8b = r8[:, g:g + 1].broadcast_to([128, 256])
                    for W_, W2_ in ((Wr, Wr2), (Wi, Wi2)):
                        S.op("dve", lambda W_=W_, W2_=W2_, gi_=gi_, r8b=r8b: nc.vector.tensor_tensor_scan(
                            W2_[:, gi_, :], r8b, W_[:, gi_, :], 0.0, ALU.mult, ALU.add),
                            reads=[r8b, W_[:, gi_, :]], writes=[W2_[:, gi_, :]])
                if STOP[0] == 26:
                    return
                for d_ in range(2):
                    pp = slice(64 * d_, 64 * d_ + 64)
                    a_, b_ = tq[2][pp], tq[3][pp]
                    if d_ == 0:
                        oxr, oxi = Xr[pp, :, 1:257], Xi[pp, :, 1:257]
                        rd_ = lambda t_: t_
                    else:
                        oxr, oxi = Xr[pp, :, 0:255], Xi[pp, :, 0:255]
                        rd_ = lambda t_: t_[:, :, 254::-1]
                    tt("dve", a_, Wr2[pp], cs_t[pp], ALU.mult)
                    tt("pool", b_, Wi2[pp], sn_t[pp], ALU.mult)
                    tt("dve", oxr, rd_(a_), rd_(b_), ALU.subtract)
                    tt("dve", a_, Wr2[pp], sn_t[pp], ALU.mult)
                    tt("pool", b_, Wi2[pp], cs_t[pp], ALU.mult)
                    tt("dve", oxi, rd_(a_), rd_(b_), ALU.add)
                if STOP[0] == 31:
                    return
                for gi_ in range(NB):
                    g = gb * NB + gi_
                    bank = ps[4 + gi_ % 2][:, 0:256]
                    mm(bank, M_sb[:, g, :], U[:, g, :], start=True, stop=False, sig=False)
                    mm(bank, WX_sb[:, g, 0, :], Xr[:, gi_, 0:256], start=False, stop=False, sig=False)
                    mm(bank, WX_sb[:, g, 1, :], Xi[:, gi_, 0:256], start=False, stop=True)
                    if STOP[0] == 32:
                        return
                    act(g1, bank, AF.Square)
                    ts("dve", g1, g1, 0.044715, 1.0, ALU.mult, ALU.add)
                    tt("dve", g1, g1, bank, ALU.mult)
                    act(g1, g1, AF.Sigmoid, scale=1.5957691216057308)
                    tt("dve", Yg, g1, bank, ALU.mult)
                    if STOP[0] == 33:
                        return
                    for cb in range(2):
                        tr(ptr_bf[:, 512 + cb * 128:512 + (cb + 1) * 128], Yg[:, cb * 128:(cb + 1) * 128], ident_bf)
                    if STOP[0] == 34:
                        return
                    for cb in range(2):
                        cp("dve", Y8[:, cb, :, 16 * g:16 * g + 16],
                           ptr_bf[:, 512 + cb * 128:512 + (cb + 1) * 128].rearrange("p (s i) -> p s i", s=8))
            if STOP[0] == 27:
                return
            ysT = V(o_run, [128, 2, L], BF16)
            for cb in range(2):
                for m in range(2):
                    for s in range(8):
                        tr(ptr_bf[:, s * 128:(s + 1) * 128], Y8[:, cb, s, m * 128:(m + 1) * 128], ident_bf)
                    cp("dve", ysT[:, m, 1024 * cb:1024 * (cb + 1)].rearrange("p (c s) -> p s c", s=8),
                       ptr_bf.rearrange("p (s c) -> p s c", s=8))
            if STOP[0] == 28:
                return
            wgl = V(O_SCR, [128, 2, 256], BF16)
            sgb = [V(O_SCR + 1024 + i * 2048, [128, 512]) for i in range(2)]
            tmp = O_SCR + 5120
            assert tmp + 8192 <= o_run
            yss = V(o_run + 16384, [128, 2, L])
            assert o_run + 32768 <= SCR_END
            S.dma("pool", wgl, wglu_d[l].rearrange("(k p) n -> p k n", p=128))
            for m in range(2):
                for tb in range(4):
                    sl = slice(tb * 512, (tb + 1) * 512)
                    bank = ps[(m * 4 + tb) % 4]
                    for k in range(2):
                        mm(bank, wgl[:, k, m * 128:(m + 1) * 128], ysT[:, k, sl], start=(k == 0), stop=(k == 1))
                    sg = sgb[tb % 2]
                    act(sg, bank, AF.Sigmoid, bias=pvs[:, l, PV_BGLU + m:PV_BGLU + m + 1])
                    tt("dve", yss[:, m, sl], ysT[:, m, sl], sg, ALU.mult)
            gnorm_fm(yss, 3, l, tmp)


        def gu_unit(wg, wu, nf, aT, sgb):
            bi = 0
            for f in range(nf):
                for tb in range(4):
                    sl = slice(tb * 512, (tb + 1) * 512)
                    bg = ps[bi % 2]
                    bu = ps[2 + bi % 2]
                    bi += 1
                    for k in range(8):
                        mm(bg, wg[:, k, f * 128:(f + 1) * 128], hT[:, k, sl], start=(k == 0), stop=(k == 7))
                    sg = sgb[bi % 2]
                    act(sg, bg, AF.Silu)
                    for k in range(8):
                        mm(bu, wu[:, k, f * 128:(f + 1) * 128], hT[:, k, sl], start=(k == 0), stop=(k == 7))
                    tt("dve", aT[:, f, sl], bu, sg, ALU.mult)

        def ffn_dense(l):
            norm_fm(PV_N2, l, O_SCR + 49152)
            aTs = [V(O_YT + i * 16384, [128, 4, L], BF16) for i in range(2)]
            rg = WRing(O_SCR, 2, [128, 8, 512])
            ru = WRing(O_SCR + 16384, 2, [128, 8, 512])
            rd = WRing(O_SCR + 32768, 2, [128, 4, 1024])
            sgb = [V(O_SCR + 49152 + i * 1024, [128, 512], BF16) for i in range(2)]
            fgv = fg_d.rearrange("(k p) n -> p k n", p=128)
            fuv = fu_d.rearrange("(k p) n -> p k n", p=128)
            fdv = fd_d.rearrange("(f p) n -> p f n", p=128)
            units = [(i * 4, 4) for i in range(5)] + [(20, 2)]
            nu = len(units)
            bufs = {}

            def ld_gu(u):
                f0, nf = units[u]
                wg = rg.load(fgv[:, :, f0 * 128:(f0 + nf) * 128], sub=lambda b: b[:, :, 0:nf * 128])
                wu = ru.load(fuv[:, :, f0 * 128:(f0 + nf) * 128], sub=lambda b: b[:, :, 0:nf * 128])
                bufs[("gu", u)] = (wg, wu)

            def ld_d(u):
                f0, nf = units[u]
                bufs[("d", u)] = rd.load(fdv[:, f0:f0 + nf, :], sub=lambda b: b[:, 0:nf, :])

            def down(u):
                f0, nf = units[u]
                wd = bufs.pop(("d", u))
                aT = aTs[u % 2]
                bi = 0
                for m in range(8):
                    for tb in range(4):
                        sl = slice(tb * 512, (tb + 1) * 512)
                        bank = ps[4 + bi % 3]
                        bi += 1
                        for f in range(nf):
                            mm(bank, wd[:, f, m * 128:(m + 1) * 128], aT[:, f, sl], start=(f == 0), stop=(f == nf - 1))
                        tt("dve" if bi % 2 == 0 else "dve", xT[:, m, sl], xT[:, m, sl], bank, ALU.add)

            ld_gu(0); ld_gu(1); ld_d(0); ld_d(1)
            for u in range(nu):
                wg, wu = bufs.pop(("gu", u))
                gu_unit(wg, wu, units[u][1], aTs[u % 2], sgb)
                if u + 2 < nu:
                    ld_gu(u + 2)
                if u >= 1:
                    down(u - 1)
                    if u + 1 < nu:
                        ld_d(u + 1)
            down(nu - 1)

        def moe_and_final(l):
            norm_fm(PV_N2, l, O_SCR + 49152)
            O_XTOK = O_YT
            x_tok = V(O_XTOK, [128, 16, D])
            o = O_XTOK + 65536
            rd = WRing(o, 2, [128, 4, 1024]); o += 16384
            sgb = [V(o + i * 1024, [128, 512], BF16) for i in range(2)]; o += 2048
            cw = V(o, [128, 16, 8]); o += 512
            lg = V(o, [128, 16, 8]); o += 512
            lg2 = V(o, [128, 16, 8]); o += 512
            eq1 = V(o, [128, 16, 8]); o += 512
            eq2 = V(o, [128, 16, 8]); o += 512
            wr_sb = V(o, [128, 8, 8]); o += 256
            wrg = V(o, [128, 8, 8]); o += 256
            sm_ = [V(o + i * 64, [128, 16]) for i in range(8)]; o += 512
            ssq, rstd_t, m1, m2, dd, ee, g1_, g2_ = sm_
            gfin = V(o, [128, D]); o += 4096
            junk = V(o, [128, D]); o += 4096
            assert o <= SCR_END, o
            S.dma("sp", wr_sb, wr_d)
            S.dma("sp", gfin, gfin_d)
            for t in range(16):
                for hf in range(2):
                    bank = ps[(t * 2 + hf) % 4]
                    for kk in range(4):
                        k = hf * 4 + kk
                        tr(bank[:, kk * 128:(kk + 1) * 128], xT[:, k, t * 128:(t + 1) * 128], ident_f)
                    cp("act" if hf == 0 else "dve", x_tok[:, t, hf * 512:(hf + 1) * 512], bank)
            for t in range(16):
                act(junk, x_tok[:, t, :], AF.Square, accum=ssq[:, t:t + 1])
            act(rstd_t, ssq, AF.Sqrt, bias=eps_col, scale=1.0 / D)
            recip(rstd_t, rstd_t)
            for k in range(8):
                ts("dve", wrg[:, k, :], wr_sb[:, k, :], pvs[:, l, PV_N2 + k:PV_N2 + k + 1], None, ALU.mult)
            lb = ps[7]
            for t in range(16):
                for k in range(8):
                    mm(lb[:, t * 8:(t + 1) * 8], xT[:, k, t * 128:(t + 1) * 128], wrg[:, k, :],
                       start=(k == 0), stop=(k == 7), sig=(k == 7 and t == 15), skip_group_check=True)
            b3 = lambda a_: a_.unsqueeze(2).broadcast_to([128, 16, 8])
            tt("dve", lg, lb[:, 0:128].rearrange("p (t e) -> p t e", e=8), b3(rstd_t), ALU.mult)
            S.op("dve", lambda: nc.vector.tensor_reduce(m1, lg, AX.X, ALU.max), reads=[lg], writes=[m1])
            tt("dve", eq1, lg, b3(m1), ALU.is_equal)
            stt("dve", lg2, eq1, -1e30, lg, ALU.mult, ALU.add)
            S.op("dve", lambda: nc.vector.tensor_reduce(m2, lg2, AX.X, ALU.max), reads=[lg2], writes=[m2])
            tt("dve", eq2, lg2, b3(m2), ALU.is_equal)
            tt("dve", dd, m2, m1, ALU.subtract)
            act(ee, dd, AF.Exp)
            ts("dve", g1_, ee, 1.0, None, ALU.add)
            recip(g1_, g1_)
            tt("dve", g2_, ee, g1_, ALU.mult)
            tt("dve", eq1, eq1, b3(g1_), ALU.mult)
            tt("dve", eq2, eq2, b3(g2_), ALU.mult)
            tt("dve", cw, eq1, eq2, ALU.add)
            aTs = [V(O_XT + i * 16384, [128, 4, L], BF16) for i in range(2)]
            rg = WRing(O_XT + 32768, 2, [128, 8, 512])
            ru = WRing(O_XT + 49152, 2, [128, 8, 512])
            NU = 7
            units = [(e, u) for e in range(NE) for u in range(NU)]
            bufs = {}

            def ld_gu(n):
                e, u = units[n]
                wg = rg.load(mg_d[e].rearrange("(k p) n -> p k n", p=128)[:, :, u * 512:(u + 1) * 512])
                wu = ru.load(mu_d[e].rearrange("(k p) n -> p k n", p=128)[:, :, u * 512:(u + 1) * 512])
                bufs[("gu", n)] = (wg, wu)

            def ld_d(n):
                e, u = units[n]
                bufs[("d", n)] = rd.load(md_d[e].rearrange("(f p) n -> p f n", p=128)[:, u * 4:(u + 1) * 4, :])

            def down(n):
                e, u = units[n]
                wd = bufs.pop(("d", n))
                aT = aTs[n % 2]
                bi = 0
                for t in range(16):
                    for hf in range(2):
                        bank = ps[4 + bi % 3]
                        bi += 1
                        for f in range(4):
                            mm(bank, aT[:, f, t * 128:(t + 1) * 128], wd[:, f, hf * 512:(hf + 1) * 512],
                               start=(f == 0), stop=(f == 3))
                        xs = x_tok[:, t, hf * 512:(hf + 1) * 512]
                        stt("dve", xs, bank, cw[:, t, e:e + 1], xs, ALU.mult, ALU.add)

            nun = len(units)
            ld_gu(0); ld_gu(1); ld_d(0); ld_d(1)
            for n in range(nun):
                wg, wu = bufs.pop(("gu", n))
                gu_unit(wg, wu, 4, aTs[n % 2], sgb)
                if n + 2 < nun:
                    ld_gu(n + 2)
                if n >= 1:
                    down(n - 1)
                    if n + 1 < nun:
                        ld_d(n + 1)
            down(nun - 1)
            for t in range(16):
                act(junk, x_tok[:, t, :], AF.Square, accum=ssq[:, t:t + 1])
            act(rstd_t, ssq, AF.Sqrt, bias=eps_col, scale=1.0 / D)
            recip(rstd_t, rstd_t)
            for t in range(16):
                stt("dve", x_tok[:, t, :], x_tok[:, t, :], rstd_t[:, t:t + 1], gfin, ALU.mult, ALU.mult)
                S.dma("sp", out_d[t * 128:(t + 1) * 128, :], x_tok[:, t, :])

        stop_after = STOP[0]
        for l in range(nlayers):
            winv = win_d[l].rearrange("(k p) n -> p k n", p=128)
            woutv = wout_d[l].rearrange("(k p) n -> p k n", p=128)
            norm_fm(PV_N1, l, O_SCR + 16384)
            if dbg:
                dump(hT, 8)
            ring = WRing(O_SCR, 3, [128, 8, 256])
            if stop_after == 0:
                break
            mixer_attention(l, winv, ring)
            if stop_after in (1, 10, 11, 12, 13, 14):
                dump(yT, 8); break
            mixer_sconv(l, winv, ring)
            if stop_after == 2:
                dump(yT, 8); break
            mixer_conformer(l, winv, ring)
            if stop_after == 3:
                dump(yT, 8); break
            mixer_s5(l, winv, ring)
            if stop_after == 4 or 20 <= stop_after < 40:
                dump(yT, 8); break
            if dbg:
                dump(yT, 8)

            def add_res(m, tb, bank):
                sl = slice(tb * 512, (tb + 1) * 512)
                tt("dve", xT[:, m, sl], xT[:, m, sl], bank, ALU.add)
            proj_fm(woutv, 0, D, yT, 8, add_res, ring, ps[0:4])
            if dbg:
                dump(xT, 8)
            if l % 2 == 0:
                ffn_dense(l)
                if dbg:
                    dump(xT, 8)
                if nlayers == 1:
                    for k in range(8):
                        S.dma("sp", out_d.rearrange("(k p) d -> p k d", p=128)[:, k, :], xT[:, k, 0:1024])
            else:
                moe_and_final(l)
        S.wait_all_dma("sp")
        print(f"[build] inst={S.n_inst} waits={S.n_wait} sems={S.nsem}", flush=True)
    return nc


_CACHE = {}


def kernel(**inputs):
    sh, per_core = host_prep(inputs)
    if "nc" not in _CACHE:
        _CACHE["nc"] = build_program(2, False)
    nc = _CACHE["nc"]
    in_maps = []
    for c in range(NCORES):
        m = dict(sh)
        m["xT"] = per_core[c]
        in_maps.append(m)
    res = run_bass_kernel_spmd(nc, in_maps, core_ids=list(range(NCORES)))
    out = np.stack([np.asarray(res.results[c]["out"], np.float32) for c in range(NCORES)], 0)
    return out
```

```python
import math
import contextlib
import numpy as np
import concourse.bass as bass
import concourse.mybir as mybir
from concourse.bass_utils import run_bass_kernel_spmd

F32 = mybir.dt.float32
BF16 = mybir.dt.bfloat16
I32 = mybir.dt.int32
ALU = mybir.AluOpType
AF = mybir.ActivationFunctionType
AX = mybir.AxisListType

_DT_SIZE = {}


def dt_size(dt):
    s = str(dt)
    if "64" in s:
        return 8
    if "32" in s:
        return 4
    if "16" in s:
        return 2
    return 1


class Sync:
    EPOCH = 24000
    DMA_RING = 6

    def __init__(self, nc, stack):
        self.nc = nc
        self.stack = stack
        self.eng = dict(pe=nc.tensor, act=nc.scalar, dve=nc.vector, pool=nc.gpsimd, sp=nc.sync)
        self.sem = {}
        self.cnt = {}
        self.pend = {}
        self.nsem = 0
        self.waited = {e: {} for e in self.eng}
        self.wr = {}
        self.rd = {}
        self.semobj = {}
        for e in ("pe", "act", "dve", "pool"):
            self._new_sem(e)
        self.ring = {}
        self.ring_pos = {}
        for q in ("sp", "pool", "act"):
            self.ring[q] = []
            self.ring_pos[q] = 0
        self.n_inst = 0
        self.n_wait = 0

    def _alloc_sem(self, name):
        s = self.stack.enter_context(self.nc.semaphore(f"{name}_{self.nsem}"))
        self.nsem += 1
        self.semobj[id(s)] = s
        return s

    def _new_sem(self, e):
        self.sem[e] = self._alloc_sem("s_" + e)
        self.cnt[e] = 0
        self.pend[e] = False

    @staticmethod
    def region(ap):
        t = ap.tensor
        dims = ap.ap
        pstep, pcount = dims[0]
        off = int(ap.offset)
        if pstep > 0:
            p0 = off // pstep
            foff = off - p0 * pstep
        else:
            p0 = 0
            foff = off
        lo = hi = foff
        for st, c in dims[1:]:
            if c <= 0:
                continue
            d = st * (c - 1)
            if d < 0:
                lo += d
            else:
                hi += d
        sz = dt_size(ap.dtype)
        if t.name.startswith("ps"):
            blo = (lo * sz) // 2048 * 2048
            bhi = ((hi + 1) * sz + 2047) // 2048 * 2048
            return (t.name, 0, 128, blo, bhi)
        return (t.name, p0, p0 + pcount, lo * sz, (hi + 1) * sz)

    @staticmethod
    def _ov(a, r):
        return a[0] < r[2] and r[1] < a[1] and a[2] < r[4] and r[3] < a[3]

    @staticmethod
    def _contained(a, r):
        return a[0] >= r[1] and a[1] <= r[2] and a[2] >= r[3] and a[3] <= r[4]

    def _need(self, e, sem, val, deps):
        k = id(sem)
        if self.waited[e].get(k, 0) >= val:
            return
        if deps.get(k, (None, 0))[1] < val:
            deps[k] = (sem, val)

    def _collect(self, e, reads, writes, skip_sem=None):
        deps = {}
        for ap in reads:
            r = self.region(ap)
            for a in self.wr.get(r[0], ()):
                if self._ov(a, r) and a[4] is not skip_sem:
                    self._need(e, a[4], a[5], deps)
        for ap in writes:
            r = self.region(ap)
            for a in self.wr.get(r[0], ()):
                if self._ov(a, r) and a[4] is not skip_sem:
                    self._need(e, a[4], a[5], deps)
            for a in self.rd.get(r[0], ()):
                if self._ov(a, r) and a[4] is not skip_sem:
                    self._need(e, a[4], a[5], deps)
        return deps

    def _emit_waits(self, e, deps):
        for k, (sem, val) in deps.items():
            for en, s in self.sem.items():
                if s is sem and val > self.cnt[en]:
                    raise RuntimeError(f"wait on unsignaled work of {en}: {val} > {self.cnt[en]}")
            self.eng[e].wait_ge(sem, val)
            self.waited[e][k] = val
            self.n_wait += 1

    def _record(self, reads, writes, sem, val):
        for ap in reads:
            r = self.region(ap)
            lst = self.rd.setdefault(r[0], [])
            lst[:] = [a for a in lst if not (a[4] is sem and self._contained(a, r))]
            lst.append((r[1], r[2], r[3], r[4], sem, val))
        for ap in writes:
            r = self.region(ap)
            lst = self.wr.setdefault(r[0], [])
            lst[:] = [a for a in lst if not self._contained(a, r)]
            lst.append((r[1], r[2], r[3], r[4], sem, val))
            lst2 = self.rd.get(r[0])
            if lst2:
                lst2[:] = [a for a in lst2 if not self._contained(a, r)]

    def op(self, e, fn, reads=(), writes=(), signal=True):
        reads = [a for a in reads if a is not None and not isinstance(a, (int, float))]
        skip = self.sem[e] if e == "pe" else None
        deps = self._collect(e, reads, writes, skip_sem=skip)
        self._emit_waits(e, deps)
        ins = fn()
        self.n_inst += 1
        if signal:
            if self.cnt[e] >= self.EPOCH and not self.pend[e]:
                old = self.sem[e]
                oldc = self.cnt[e]
                self._new_sem(e)
            self.cnt[e] += 1
            ins.then_inc(self.sem[e], 1)
            self.pend[e] = False
            self._record(reads, writes, self.sem[e], self.cnt[e])
        else:
            self.pend[e] = True
            self._record(reads, writes, self.sem[e], self.cnt[e] + 1)
        return ins

    def dma(self, q, out, in_, **kw):
        reads = [in_] if str(in_.space) != "DRAM" and "DRAM" not in str(in_.space).upper() else []
        writes = [out] if "DRAM" not in str(out.space).upper() else []
        ring = self.ring[q]
        pos = self.ring_pos[q]
        if len(ring) < self.DMA_RING:
            ring.append([self._alloc_sem("d_" + q), 0])
        slot = ring[pos % self.DMA_RING]
        self.ring_pos[q] = pos + 1
        deps = self._collect(q, reads, writes)
        if slot[1] > 0:
            self._need(q, slot[0], slot[1], deps)
        if slot[1] >= self.EPOCH * 2:
            self._emit_waits(q, deps)
            deps = {}
            slot[0] = self._alloc_sem("d_" + q)
            slot[1] = 0
        self._emit_waits(q, deps)
        ins = self.eng[q].dma_start(out=out, in_=in_, **kw)
        slot[1] += 16
        ins.then_inc(slot[0], 16)
        self.n_inst += 1
        self._record(reads, writes, slot[0], slot[1])
        return ins

    def wait_all_dma(self, e="sp"):
        for q, ring in self.ring.items():
            for sem, val in ring:
                if val > 0 and self.waited[e].get(id(sem), 0) < val:
                    self.eng[e].wait_ge(sem, val)
                    self.waited[e][id(sem)] = val


D = 1024
L = 2048
NCORES = 8
GRID_W = 64
ROWS = 32
NEG = -30000.0
FF_D = 2816
FF_E = 3584
NE = 8
EPS = 1e-6

PV_N1 = 0
PV_N2 = 8
PV_GN = 16
PV_SCW = 24
PV_CFW = 30
PV_CFB = 92
PV_LNG = 94
PV_LNB = 96
PV_BGLU = 98
PV_COLS = 100

C_ONES = 0
C_ID = 128
C_MF = 256
C_MB = 384
C_KK = 512
C_CIDX = 545
C_COLS = 801


def _row_start(r):
    return int(np.clip(r - 4, 0, ROWS - 8))


def na_plan():
    plans = []
    keys = {}
    for i in range(16):
        r0 = 2 * i
        lo = min(_row_start(r0), _row_start(r0 + 1))
        hi = max(_row_start(r0), _row_start(r0 + 1)) + 7
        a0, a1 = lo // 2, hi // 2
        lst = []
        for a in range(a0, a1 + 1):
            key = (2 * a - r0, _row_start(r0) - r0, _row_start(r0 + 1) - r0)
            if key not in keys:
                keys[key] = len(keys)
            lst.append((a, keys[key]))
        plans.append(lst)
    return plans, keys


def na_tables(rpb):
    plans, keys = na_plan()
    out = np.empty((len(keys), 128, 4, 128), np.float32)
    kl = np.arange(128)
    kr_l, kc = kl // 64, kl % 64
    ql = np.arange(128)
    qr_l, qc = ql // 64, ql % 64
    qcs = np.clip(qc - 8, 0, GRID_W - 16)
    for (da, rs0, rs1), idx in keys.items():
        krow = (da + kr_l)[:, None]
        qrow = qr_l[None, :]
        rs = np.where(qr_l == 0, rs0, rs1)[None, :]
        row_ok = (krow >= rs) & (krow < rs + 8)
        col_ok = (kc[:, None] >= qcs[None, :]) & (kc[:, None] < qcs[None, :] + 16)
        dr = np.clip(krow - qrow + 7, 0, 14)
        dc = np.clip(kc[:, None] - qc[None, :] + 15, 0, 30)
        ok = row_ok & col_ok
        for h in range(4):
            out[idx, :, h, :] = np.where(ok, rpb[h][dr, dc], np.float32(NEG))
    return out.reshape(len(keys), 128, 512)


def fm_cols(v, nchunk):
    return np.ascontiguousarray(np.asarray(v, np.float32).reshape(nchunk, 128).T)


def make_consts():
    c = np.zeros((128, C_COLS), np.float32)
    c[:, C_ONES:C_ONES + 128] = 1.0
    c[:, C_ID:C_ID + 128] = np.eye(128, dtype=np.float32)
    sp = np.arange(128)[:, None] // 16
    s = np.arange(128)[None, :] // 16
    c[:, C_MF:C_MF + 128] = (sp <= s)
    c[:, C_MB:C_MB + 128] = (sp >= s)
    kk = np.zeros((128, 33), np.float32)
    sv = np.arange(8)
    kk[:64, 0:8] = -sv
    kk[64:, 0:8] = sv
    kk[:64, 8:16] = 7 - sv
    kk[64:, 8:16] = sv
    kk[:64, 16:24] = sv
    kk[64:, 16:24] = -sv
    kk[:64, 24:32] = sv + 1
    kk[64:, 24:32] = 8 - sv
    kk[:, 32] = 1
    c[:, C_KK:C_KK + 33] = kk
    c[:, C_CIDX:C_CIDX + 256] = np.arange(256)[None, :]
    return c


def host_prep(inp):
    f = lambda a: np.ascontiguousarray(np.asarray(a, np.float32))
    sh = {}
    pv = np.zeros((2, 128, PV_COLS), np.float32)
    for l in range(2):
        pv[l, :, PV_N1:PV_N1 + 8] = fm_cols(inp["norm1_g"][l], 8)
        pv[l, :, PV_N2:PV_N2 + 8] = fm_cols(inp["norm2_g"][l], 8)
        pv[l, :, PV_GN:PV_GN + 8] = fm_cols(inp["grp_norm_g"][l], 8)
        for t in range(3):
            pv[l, :, PV_SCW + 2 * t:PV_SCW + 2 * t + 2] = fm_cols(inp["sc_conv_w"][l][t], 2)
        for t in range(31):
            pv[l, :, PV_CFW + 2 * t:PV_CFW + 2 * t + 2] = fm_cols(inp["cf_conv_w"][l][t], 2)
        pv[l, :, PV_CFB:PV_CFB + 2] = fm_cols(inp["cf_conv_b"][l], 2)
        pv[l, :, PV_LNG:PV_LNG + 2] = fm_cols(inp["cf_ln_g"][l], 2)
        pv[l, :, PV_LNB:PV_LNB + 2] = fm_cols(inp["cf_ln_b"][l], 2)
        pv[l, :, PV_BGLU:PV_BGLU + 2] = fm_cols(inp["ssm_b_glu"][l], 2)
    sh["pv"] = pv
    sh["consts"] = make_consts()
    sh["natab"] = np.stack([na_tables(np.asarray(inp["na_rpb"][l], np.float32)) for l in range(2)])
    a_re = f(inp["ssm_a_re"])
    a_im = f(inp["ssm_a_im"])
    ldt = f(inp["ssm_log_dt"])
    s5a = np.zeros((2, 128, 16, 3), np.float32)
    s5a[..., 0] = a_re.transpose(0, 1, 3, 2).reshape(2, 128, 16)
    s5a[..., 1] = a_im.transpose(0, 1, 3, 2).reshape(2, 128, 16)
    s5a[..., 2] = np.broadcast_to(ldt[:, :, None, :], (2, 2, 64, 16)).reshape(2, 128, 16)
    sh["s5a"] = s5a
    b_re = f(inp["ssm_b_re"])
    b_im = f(inp["ssm_b_im"])
    sh["s5b"] = np.ascontiguousarray(np.stack([b_re, b_im], 0).transpose(1, 2, 4, 3, 0, 5).reshape(2, 128, 16, 2, 16))
    c_re = f(inp["ssm_c_re"])
    c_im = f(inp["ssm_c_im"])
    sh["s5c"] = np.ascontiguousarray(np.stack([c_re, c_im], 0).transpose(1, 2, 5, 3, 0, 4).reshape(2, 128, 16, 2, 16))
    d = f(inp["ssm_d"])
    dd = d.reshape(2, 16, 16)
    sh["s5d"] = np.ascontiguousarray(np.broadcast_to(dd.transpose(0, 2, 1)[:, None, :, :], (2, 8, 16, 16)).reshape(2, 128, 16))
    sh["wglu"] = f(inp["ssm_w_glu"])
    sh["win"] = f(inp["w_in"])
    sh["wout"] = f(inp["w_out"])
    sh["fg"] = f(inp["ffn_w_gate"][0])
    sh["fu"] = f(inp["ffn_w_up"][0])
    sh["fd"] = f(inp["ffn_w_down"][0])
    sh["mg"] = f(inp["moe_w_gate"][0])
    sh["mu"] = f(inp["moe_w_up"][0])
    sh["md"] = f(inp["moe_w_down"][0])
    sh["wr"] = np.ascontiguousarray(f(inp["moe_w_router"][0]).reshape(8, 128, 8).transpose(1, 0, 2))
    sh["gfin"] = np.ascontiguousarray(np.broadcast_to(f(inp["final_norm_g"])[None, :], (128, 1024)))
    x = f(inp["x"])
    per_core = [np.ascontiguousarray(x[b].T) for b in range(NCORES)]
    return sh, per_core


ARENA_BYTES = 212800
O_XT = 0
O_HT = 65536
O_YT = 98304
O_SCR = 131072
O_CONST = 208000
SCR_END = O_CONST
STOP = [99]


def build_program(nlayers=2, dbg=False):
    nc = bass.Bass("TRN2", target_bir_lowering=False)
    dr = {}

    def din(name, shape):
        dr[name] = nc.dram_tensor(name, list(shape), F32, kind="ExternalInput").ap()
        return dr[name]

    xT_d = din("xT", [D, L])
    win_d = din("win", [2, D, 2304])
    wout_d = din("wout", [2, D, D])
    fg_d = din("fg", [D, FF_D])
    fu_d = din("fu", [D, FF_D])
    fd_d = din("fd", [FF_D, D])
    mg_d = din("mg", [NE, D, FF_E])
    mu_d = din("mu", [NE, D, FF_E])
    md_d = din("md", [NE, FF_E, D])
    wr_d = din("wr", [128, 8, 8])
    pv_d = din("pv", [2, 128, PV_COLS])
    consts_d = din("consts", [128, C_COLS])
    plans, tkeys = na_plan()
    NT = len(tkeys)
    natab_d = din("natab", [2, NT, 128, 512])
    s5a_d = din("s5a", [2, 128, 16, 3])
    s5b_d = din("s5b", [2, 128, 16, 2, 16])
    s5c_d = din("s5c", [2, 128, 16, 2, 16])
    s5d_d = din("s5d", [2, 128, 16])
    wglu_d = din("wglu", [2, 256, 256])
    gfin_d = din("gfin", [128, 1024])
    out_d = nc.dram_tensor("out", [L, D], F32, kind="ExternalOutput").ap()
    dbg_d = None
    if dbg:
        dbg_d = nc.dram_tensor("dbg", [8, 128, 8, 2048], F32, kind="ExternalOutput").ap()

    st = contextlib.ExitStack()
    with st:
        S = Sync(nc, st)
        arena_t = st.enter_context(nc.sbuf_tensor("arena", [128, ARENA_BYTES // 4], F32))
        arena = arena_t[:]
        psA = st.enter_context(nc.psum_tensor("psA", [128, 1024], F32))[:]
        psB = st.enter_context(nc.psum_tensor("psB", [128, 1024], F32))[:]
        ps = [psA[:, 0:512], psA[:, 512:1024], psB[:, 0:512], psB[:, 512:1024]]
        ps += [st.enter_context(nc.psum_tensor(f"ps{i}", [128, 512], F32))[:] for i in range(4, 8)]
        EN = dict(dve=nc.vector, pool=nc.gpsimd)

        def V(off, shape, dt=F32):
            assert off % 4 == 0
            n = 1
            for s_ in shape[1:]:
                n *= s_
            nb = n * dt_size(dt)
            assert nb % 4 == 0
            ap = arena[:, off // 4: off // 4 + nb // 4]
            if dt != F32:
                ap = ap.bitcast(dt)
            if len(shape) == 3:
                ap = ap.rearrange("p (a b) -> p a b", a=shape[1])
            elif len(shape) == 4:
                ap = ap.rearrange("p (a b c) -> p a b c", a=shape[1], b=shape[2])
            elif len(shape) == 5:
                ap = ap.rearrange("p (a b c d) -> p a b c d", a=shape[1], b=shape[2], c=shape[3])
            return ap

        def mm(out, lhsT, rhs, start=True, stop=True, sig=None, **kw):
            if lhsT.partition_size() <= 64 and "tile_position" not in kw:
                kw["tile_position"] = (lhsT.base_partition(), 0)
            S.op("pe", lambda: nc.tensor.matmul(out, lhsT, rhs, start=start, stop=stop, **kw),
                 reads=[lhsT, rhs], writes=[out], signal=(stop if sig is None else sig))

        def tr(out, in_, ident):
            S.op("pe", lambda: nc.tensor.transpose(out, in_, ident), reads=[in_, ident], writes=[out])

        def act(out, in_, func, bias=None, scale=None, accum=None):
            kw = {}
            if bias is not None:
                kw["bias"] = bias
            if scale is not None:
                kw["scale"] = scale
            if accum is not None:
                kw["accum_out"] = accum
            S.op("act", lambda: nc.scalar.activation(out, in_, func, **kw),
                 reads=[in_, bias, scale], writes=[out] + ([accum] if accum is not None else []))

        def tt(e, out, a, b, op):
            S.op(e, lambda: EN[e].tensor_tensor(out, a, b, op), reads=[a, b], writes=[out])

        def ts(e, out, a, s1, s2, op0, op1=None):
            if op1 is None:
                S.op(e, lambda: EN[e].tensor_scalar(out, a, s1, None, op0), reads=[a, s1], writes=[out])
            else:
                S.op(e, lambda: EN[e].tensor_scalar(out, a, s1, s2, op0, op1), reads=[a, s1, s2], writes=[out])

        def stt(e, out, a, s, b, op0, op1):
            e = "dve"
            S.op(e, lambda: EN[e].scalar_tensor_tensor(out, a, s, b, op0, op1), reads=[a, s, b], writes=[out])

        def cp(e, out, a):
            if e == "act":
                S.op("act", lambda: nc.scalar.copy(out, a), reads=[a], writes=[out])
            else:
                S.op(e, lambda: EN[e].tensor_copy(out, a), reads=[a], writes=[out])

        def memset(e, out, val):
            S.op(e, lambda: EN[e].memset(out, val), reads=[], writes=[out])

        def recip(out, a):
            S.op("dve", lambda: nc.vector.reciprocal(out, a), reads=[a], writes=[out])

        dbg_slot = [0]

        def dump(ap_fm, nchunk, cast=False):
            if not dbg:
                return
            i = dbg_slot[0]
            dbg_slot[0] += 1
            for k in range(nchunk):
                S.dma("pool", dbg_d[i, :, k, :], ap_fm[:, k, :])

        xT = V(O_XT, [128, 8, L])
        hT = V(O_HT, [128, 8, L], BF16)
        yT = V(O_YT, [128, 8, L], BF16)
        consts = V(O_CONST, [128, C_COLS])
        pvs = V(O_CONST + 3204, [128, 2, PV_COLS])
        o_misc = O_CONST + 3204 + 800
        ident_bf = V(o_misc, [128, 128], BF16)
        ones_bf = V(o_misc + 256, [128, 128], BF16)
        small = V(o_misc + 512, [128, 64])
        assert o_misc + 768 <= ARENA_BYTES
        ones_f = consts[:, C_ONES:C_ONES + 128]
        ident_f = consts[:, C_ID:C_ID + 128]

        S.dma("sp", consts, consts_d)
        for l in range(2):
            S.dma("sp", pvs[:, l, :], pv_d[l])
        xTv = xT_d.rearrange("(k p) t -> p k t", p=128)
        for k in range(8):
            S.dma("sp", xT[:, k, :], xTv[:, k, :])
        cp("dve", ident_bf, ident_f)
        cp("dve", ones_bf, ones_f)

        def rstd_block(chunks, inv_n, rstd_out, bank, sqa, sqb):
            nk = len(chunks)
            for k, c in enumerate(chunks):
                sq = sqa if k % 2 == 0 else sqb
                act(sq, c, AF.Square)
                mm(bank, ones_f, sq, start=(k == 0), stop=(k == nk - 1), sig=True)
            act(rstd_out, bank, AF.Sqrt, bias=eps_col, scale=inv_n)
            recip(rstd_out, rstd_out)

        eps_col = small[:, 0:1]
        memset("dve", eps_col, EPS)

        def norm_fm(gcol0, l, o_tmp):
            sqa = V(o_tmp, [128, 512])
            sqb = V(o_tmp + 2048, [128, 512])
            for tb in range(4):
                rs = V(o_tmp + 4096 + (tb % 2) * 2048, [128, 512])
                sl = slice(tb * 512, (tb + 1) * 512)
                rstd_block([xT[:, k, sl] for k in range(8)], 1.0 / D, rs, ps[7], sqa, sqb)
                for k in range(8):
                    stt("dve" if k % 2 == 0 else "pool", hT[:, k, sl], xT[:, k, sl],
                        pvs[:, l, gcol0 + k:gcol0 + k + 1], rs, ALU.mult, ALU.mult)

        def gnorm_fm(src, gi, l, o_tmp):
            sqa = V(o_tmp, [128, 512])
            sqb = V(o_tmp + 2048, [128, 512])
            for tb in range(4):
                rs = V(o_tmp + 4096 + (tb % 2) * 2048, [128, 512])
                sl = slice(tb * 512, (tb + 1) * 512)
                rstd_block([src[:, c, sl] for c in range(2)], 1.0 / 256, rs, ps[7], sqa, sqb)
                for c in range(2):
                    gc = PV_GN + 2 * gi + c
                    stt("dve", yT[:, 2 * gi + c, sl], src[:, c, sl], pvs[:, l, gc:gc + 1], rs, ALU.mult, ALU.mult)

        class WRing:
            def __init__(self, off, nbuf, shape):
                n = 1
                for s_ in shape[1:]:
                    n *= s_
                self.bufs = [V(off + i * n * 2, shape, BF16) for i in range(nbuf)]
                self.i = 0
                self.nbytes = nbuf * n * 2

            def load(self, src, sub=None):
                b = self.bufs[self.i % len(self.bufs)]
                self.i += 1
                dst = b if sub is None else sub(b)
                if len(src.shape) == 3 and src.shape[1] > 1 and src.shape[2] * 4 >= 2048:
                    half = src.shape[1] // 2
                    S.dma("pool", dst[:, 0:half, :], src[:, 0:half, :])
                    S.dma("pool", dst[:, half:, :], src[:, half:, :])
                elif len(src.shape) == 3 and src.shape[1] > 1:
                    for k in range(src.shape[1]):
                        S.dma("pool", dst[:, k, :], src[:, k, :])
                else:
                    S.dma("pool", dst, src)
                return b

        def proj_fm(wv, col0, ncols, actT, K, consumer, ring, banks, unit=256):
            nunits = ncols // unit
            loaded = {}

            def ld(u):
                loaded[u] = ring.load(wv[:, :, col0 + u * unit: col0 + (u + 1) * unit])
            ld(0)
            if nunits > 1:
                ld(1)
            bi = 0
            for u in range(nunits):
                if u + 2 < nunits:
                    ld(u + 2)
                wb = loaded.pop(u)
                for mi in range(unit // 128):
                    m = u * (unit // 128) + mi
                    for tb in range(4):
                        bank = banks[bi % len(banks)]
                        bi += 1
                        for k in range(K):
                            mm(bank, wb[:, k, mi * 128:(mi + 1) * 128], actT[:, k, tb * 512:(tb + 1) * 512],
                               start=(k == 0), stop=(k == K - 1))
                        consumer(m, tb, bank)

        def proj_w(wb, c0, ncols, actT, K, consumer, banks):
            bi = 0
            for m in range(ncols // 128):
                for tb in range(4):
                    bank = banks[bi % len(banks)]
                    bi += 1
                    for k in range(K):
                        mm(bank, wb[:, k, c0 + m * 128:c0 + (m + 1) * 128], actT[:, k, tb * 512:(tb + 1) * 512],
                           start=(k == 0), stop=(k == K - 1))
                    consumer(m, tb, bank)

        RING0 = SCR_END - 12288
        RING1 = SCR_END - 24576
        MIX_END = RING1
        ringbuf = [V(RING0, [128, 8, 768], BF16), V(RING1, [128, 8, 768], BF16)]

        def ring_load(slot, src):
            n = src.shape[2]
            dst = ringbuf[slot][:, :, 0:n]
            S.dma("pool", dst[:, 0:4, :], src[:, 0:4, :])
            S.dma("pool", dst[:, 4:8, :], src[:, 4:8, :])
            return ringbuf[slot]


        def mixer_attention(l, wA):
            o = O_SCR
            QT = V(o, [128, 2, L], BF16); o += 8192
            KT = V(o, [128, 2, L], BF16); o += 8192
            Vaug = V(o, [128, 16, 4, 65], BF16); o += 8320
            Etab = V(O_YT + 8192, [128, NT, 512], BF16)
            assert NT * 1024 <= 24576
            Pb = [V(o + i * 1024, [128, 512], BF16) for i in range(2)]; o += 2048
            stg = [V(o, [128, 512])] * 2; o += 2048
            ytok = V(o, [128, 4, 64]); o += 1024
            ybf = V(o, [128, 256], BF16); o += 512
            junk = stg[0][:, 0:256]
            assert o <= MIX_END, o
            ss = small[:, 1:2]
            rec4 = small[:, 4:8]

            for t in range(NT):
                S.dma("sp", stg[t % 2], natab_d[l, t])
                act(Etab[:, t, :], stg[t % 2], AF.Exp)
            memset("dve", Vaug[:, :, :, 64:65], 1.0)

            if STOP[0] == 10:
                return
            def consumer(m, tb, bank):
                dst = QT if m < 2 else KT
                cp("act", dst[:, m % 2, tb * 512:(tb + 1) * 512], bank)
            proj_w(wA, 0, 512, hT, 8, consumer, ps[0:4])
            if STOP[0] == 11:
                return
            wvb = wA[:, :, 512:768]
            for t in range(16):
                bank = ps[t % 4]
                for k in range(8):
                    mm(bank[:, 0:256], hT[:, k, t * 128:(t + 1) * 128], wvb[:, k, :], start=(k == 0), stop=(k == 7))
                cp("dve", Vaug[:, t, :, 0:64], bank[:, 0:256].rearrange("p (h d) -> p h d", h=4))
            if STOP[0] == 12:
                return
            ptr_bf = ps[7].bitcast(BF16)
            for i in range(16):
                if STOP[0] == 13 and i == 1:
                    return
                pvb = ps[6]
                pvv = pvb[:, 0:260].rearrange("p (h d) -> p h d", h=4)
                plan = plans[i]
                POS = {0: 0, 2: 1, 1: 2, 3: 3}
                for ci, (a, ti) in enumerate(plan):
                    sb0, sb1 = (ps[4], ps[5]) if ci % 2 == 0 else (ps[2], ps[3])
                    for h in range(4):
                        hp = (h % 2) * 64
                        sbh = sb0 if hp == 0 else sb1
                        mm(sbh[:, (h // 2) * 128:(h // 2 + 1) * 128], KT[hp:hp + 64, h // 2, a * 128:(a + 1) * 128],
                           QT[hp:hp + 64, h // 2, i * 128:(i + 1) * 128], start=True, stop=True, sig=True)
                    P = Pb[ci % 2]
                    Ev = Etab[:, ti, :].rearrange("p (h q) -> p h q", h=4)
                    act(P[:, 0:256], sb0[:, 0:256], AF.Exp, scale=0.125)
                    act(P[:, 256:512], sb1[:, 0:256], AF.Exp, scale=0.125)
                    tt("dve", P[:, 0:256].rearrange("p (h q) -> p h q", h=2), P[:, 0:256].rearrange("p (h q) -> p h q", h=2),
                       Ev[:, 0::2, :], ALU.mult)
                    tt("dve", P[:, 256:512].rearrange("p (h q) -> p h q", h=2), P[:, 256:512].rearrange("p (h q) -> p h q", h=2),
                       Ev[:, 1::2, :], ALU.mult)
                    for h in range(4):
                        first = (ci == 0 and h == 0)
                        last = (ci == len(plan) - 1 and h == 3)
                        mm(pvv[:, h, :], P[:, POS[h] * 128:(POS[h] + 1) * 128], Vaug[:, a, h, :], start=first, stop=last,
                           sig=(h == 3), skip_group_check=True)
                if STOP[0] == 14:
                    return
                recip(rec4, pvv[:, :, 64])
                tt("dve", ytok, pvv[:, :, 0:64], rec4.unsqueeze(2).broadcast_to([128, 4, 64]), ALU.mult)
                yflat = ytok.rearrange("p h d -> p (h d)")
                act(junk, yflat, AF.Square, accum=ss)
                act(ss, ss, AF.Sqrt, bias=eps_col, scale=1.0 / 256)
                recip(ss, ss)
                ts("dve", ybf, yflat, ss, None, ALU.mult)
                for m in range(2):
                    tr(ptr_bf[:, m * 128:(m + 1) * 128], ybf[:, m * 128:(m + 1) * 128], ident_bf)
                for m in range(2):
                    gc = PV_GN + m
                    ts("dve", yT[:, m, i * 128:(i + 1) * 128], ptr_bf[:, m * 128:(m + 1) * 128],
                       pvs[:, l, gc:gc + 1], None, ALU.mult)

        def mixer_sconv(l, wB):
            o = O_SCR
            scb = V(o, [128, 2, L], BF16); o += 8192
            scv = V(o, [128, 2, L]); o += 16384
            sco = V(o, [128, 2, L]); o += 16384
            tmp = o
            assert o + 8192 <= MIX_END

            def consumer(m, tb, bank):
                mm_ = m - 6
                sl = slice(tb * 512, (tb + 1) * 512)
                if mm_ < 2:
                    cp("act", scb[:, mm_, sl], bank)
                elif mm_ < 4:
                    cp("act", scv[:, mm_ - 2, sl], bank)
                else:
                    tt("dve", scv[:, mm_ - 4, sl], scv[:, mm_ - 4, sl], bank, ALU.mult)
            proj_w(wB, 0, 768, hT, 8, lambda m, tb, bank: consumer(m + 6, tb, bank), ps[0:4])
            for c in range(2):
                w = lambda t: pvs[:, l, PV_SCW + 2 * t + c:PV_SCW + 2 * t + c + 1]
                ts("dve", sco[:, c, :], scv[:, c, :], w(1), None, ALU.mult)
                stt("dve", sco[:, c, 1:L], scv[:, c, 0:L - 1], w(0), sco[:, c, 1:L], ALU.mult, ALU.add)
                stt("dve", sco[:, c, 0:L - 1], scv[:, c, 1:L], w(2), sco[:, c, 0:L - 1], ALU.mult, ALU.add)
                tt("dve", sco[:, c, :], sco[:, c, :], scb[:, c, :], ALU.mult)
            gnorm_fm(sco, 1, l, tmp)

        def mixer_conformer(l, wCD):
            o = O_SCR
            cfa = V(o, [128, 2, L]); o += 16384
            PADW = L + 32
            vpad = V(o, [128, 2, PADW], BF16); o += 2 * PADW * 2
            dg = V(o, [128, 2, 31, 128], BF16); o += 2 * 31 * 256
            sig = [V(o, [128, 512])] * 2; o += 2048
            ycf = cfa
            tmp = o
            assert o + 8192 <= MIX_END, o
            for c in range(2):
                memset("dve", vpad[:, c, 0:15], 0.0)
                memset("dve", vpad[:, c, 15 + L:PADW], 0.0)
                for t in range(31):
                    col = PV_CFW + 2 * t + c
                    act(dg[:, c, t, :], ident_f, AF.Identity, scale=pvs[:, l, col:col + 1])

            def consumer(m, tb, bank):
                sl = slice(tb * 512, (tb + 1) * 512)
                if m < 2:
                    cp("act", cfa[:, m, sl], bank)
                else:
                    sg = sig[tb % 2]
                    act(sg, bank, AF.Sigmoid)
                    tt("dve", vpad[:, m - 2, 15 + tb * 512:15 + (tb + 1) * 512], cfa[:, m - 2, sl], sg, ALU.mult)
            proj_w(wCD, 0, 512, hT, 8, consumer, ps[0:4])
            bi = 0
            for c in range(2):
                for tb in range(4):
                    bank = ps[bi % 4]; bi += 1
                    for t in range(31):
                        mm(bank, dg[:, c, t, :], vpad[:, c, tb * 512 + t: tb * 512 + t + 512], start=(t == 0), stop=(t == 30))
                    act(cfa[:, c, tb * 512:(tb + 1) * 512], bank, AF.Identity, bias=pvs[:, l, PV_CFB + c:PV_CFB + c + 1])
            sqa = V(tmp, [128, 512]); sqb = V(tmp + 2048, [128, 512])
            mean = V(tmp + 4096, [128, 512]); var = V(tmp + 6144, [128, 512])
            for tb in range(4):
                sl = slice(tb * 512, (tb + 1) * 512)
                for c in range(2):
                    mm(ps[6], ones_f, cfa[:, c, sl], start=(c == 0), stop=(c == 1))
                for c in range(2):
                    sq = sqa if c == 0 else sqb
                    act(sq, cfa[:, c, sl], AF.Square)
                    mm(ps[7], ones_f, sq, start=(c == 0), stop=(c == 1))
                ts("dve", mean, ps[6], 1.0 / 256, None, ALU.mult)
                tt("dve", sqa, mean, mean, ALU.mult)
                stt("dve", var, ps[7], 1.0 / 256, sqa, ALU.mult, ALU.subtract)
                act(var, var, AF.Sqrt, bias=eps_col, scale=1.0)
                recip(var, var)
                for c in range(2):
                    tt("dve", sqb, cfa[:, c, sl], mean, ALU.subtract)
                    tt("dve", sqb, sqb, var, ALU.mult)
                    act(ycf[:, c, sl], sqb, AF.Silu, bias=pvs[:, l, PV_LNB + c:PV_LNB + c + 1],
                        scale=pvs[:, l, PV_LNG + c:PV_LNG + c + 1])
            gnorm_fm(ycf, 2, l, tmp)


        PI = math.pi

        def mixer_s5(l, wCD):
            o = O_SCR
            M_sb = V(o, [128, 16, 128], BF16); o += 4096
            WS_sb = V(o, [128, 16, 2, 128], BF16); o += 8192
            WX_sb = V(o, [128, 16, 2, 128], BF16); o += 8192
            r8 = V(o, [128, 16]); o += 64
            th8 = V(o, [128, 16]); o += 64
            o_run = o
            negpi = small[:, 8:9]
            memset("dve", negpi, -PI)

            def sincos(ang, osin, ocos, tmp, tmp2):
                ts("dve", tmp.bitcast(I32), ang, 1.0 / (2 * PI), None, ALU.mult)
                cp("dve", tmp2, tmp.bitcast(I32))
                stt("dve", tmp2, tmp2, -2 * PI, ang, ALU.mult, ALU.add)
                act(tmp, tmp2, AF.Sin, scale=0.5)
                act(ocos, tmp2, AF.Sin, scale=0.25)
                tt("dve", ocos, ocos, ocos, ALU.mult)
                ts("dve", ocos, ocos, -2.0, 1.0, ALU.mult, ALU.add)
                stt("dve", osin, tmp, 2.0, ocos, ALU.mult, ALU.mult)
                tt("dve", ocos, tmp, tmp, ALU.mult)
                ts("dve", ocos, ocos, -2.0, 1.0, ALU.mult, ALU.add)

            sa = V(o, [128, 16, 3]); o += 192
            sbp = V(o, [128, 16, 2, 16]); o += 2048
            scp = V(o, [128, 16, 2, 16]); o += 2048
            sdp = V(o, [128, 16]); o += 64
            S.dma("sp", sa, s5a_d[l])
            S.dma("sp", sbp, s5b_d[l])
            S.dma("sp", scp, s5c_d[l])
            S.dma("sp", sdp, s5d_d[l])
            sm = [V(o + i * 64, [128, 16]) for i in range(10)]; o += 640
            dtv, arv, thv, nr, den, gr, gi, t0, t1, li1 = sm
            a_re = sa[:, :, 0]
            a_im = sa[:, :, 1]
            act(dtv, sa[:, :, 2], AF.Exp)
            tt("dve", arv, a_re, dtv, ALU.mult)
            tt("dve", thv, a_im, dtv, ALU.mult)
            ts("dve", t0, arv, 8.0, None, ALU.mult)
            act(r8, t0, AF.Exp)
            ts("dve", th8, thv, 8.0, None, ALU.mult)
            NS = 33
            ark = V(o, [128, 16, NS]); o += 16 * NS * 4
            thk = V(o, [128, 16, NS]); o += 16 * NS * 4
            LR = V(o, [128, 16, NS]); o += 16 * NS * 4
            LI = V(o, [128, 16, NS]); o += 16 * NS * 4
            tk = V(o, [128, 16, NS]); o += 16 * NS * 4
            tk2 = V(o, [128, 16, NS]); o += 16 * NS * 4
            KKb = consts[:, C_KK:C_KK + NS].unsqueeze(1).broadcast_to([128, 16, NS])
            tt("dve", ark, KKb, arv.unsqueeze(2).broadcast_to([128, 16, NS]), ALU.mult)
            tt("dve", thk, KKb, thv.unsqueeze(2).broadcast_to([128, 16, NS]), ALU.mult)
            act(ark, ark, AF.Exp)
            sincos(thk, LI, LR, tk, tk2)
            tt("dve", LR, LR, ark, ALU.mult)
            tt("dve", LI, LI, ark, ALU.mult)
            ts("dve", nr, LR[:, :, 32], -1.0, None, ALU.add)
            cp("dve", li1, LI[:, :, 32])
            tt("dve", den, a_re, a_re, ALU.mult)
            tt("dve", t0, a_im, a_im, ALU.mult)
            tt("dve", den, den, t0, ALU.add)
            recip(den, den)
            tt("dve", gr, nr, a_re, ALU.mult)
            tt("dve", t0, li1, a_im, ALU.mult)
            tt("dve", gr, gr, t0, ALU.add)
            tt("dve", gr, gr, den, ALU.mult)
            tt("dve", gi, li1, a_re, ALU.mult)
            tt("dve", t0, nr, a_im, ALU.mult)
            tt("dve", gi, gi, t0, ALU.subtract)
            tt("dve", gi, gi, den, ALU.mult)
            bbr = V(o, [128, 16, 16]); o += 1024
            bbi = V(o, [128, 16, 16]); o += 1024
            tb_ = V(o, [128, 16, 16]); o += 1024
            Br = sbp[:, :, 0, :]
            Bi = sbp[:, :, 1, :]
            grb = gr.unsqueeze(2).broadcast_to([128, 16, 16])
            gib = gi.unsqueeze(2).broadcast_to([128, 16, 16])
            tt("dve", bbr, Br, grb, ALU.mult)
            tt("dve", tb_, Bi, gib, ALU.mult)
            tt("dve", bbr, bbr, tb_, ALU.subtract)
            tt("dve", bbi, Bi, grb, ALU.mult)
            tt("dve", tb_, Br, gib, ALU.mult)
            tt("dve", bbi, bbi, tb_, ALU.add)
            Cr = scp[:, :, 0, :]
            Ci = scp[:, :, 1, :]
            big = [V(o + i * 2048, [128, 4, 8, 16]) for i in range(9)]; o += 9 * 2048
            assert o <= RING0, o
            Pr, Pi, WSr, WSi, Qr, Qi, WXr, WXi, tmpb = big
            MFb = consts[:, C_MF:C_MF + 128].unsqueeze(1).broadcast_to([128, 4, 128])
            MBb = consts[:, C_MB:C_MB + 128].unsqueeze(1).broadcast_to([128, 4, 128])

            def cmul(outr, outi, slot0, vr, vi, gs, neg_i=False):
                lr = LR[:, gs, slot0:slot0 + 8].unsqueeze(3).broadcast_to([128, 4, 8, 16])
                li = LI[:, gs, slot0:slot0 + 8].unsqueeze(3).broadcast_to([128, 4, 8, 16])
                vrb = vr[:, gs, :].unsqueeze(2).broadcast_to([128, 4, 8, 16])
                vib = vi[:, gs, :].unsqueeze(2).broadcast_to([128, 4, 8, 16])
                tt("dve", outr, lr, vrb, ALU.mult)
                tt("pool", tmpb, li, vib, ALU.mult)
                tt("dve", outr, outr, tmpb, ALU.subtract)
                if not neg_i:
                    tt("dve", outi, lr, vib, ALU.mult)
                    tt("pool", tmpb, li, vrb, ALU.mult)
                    tt("dve", outi, outi, tmpb, ALU.add)
                else:
                    tt("dve", outi, lr, vib, ALU.mult)
                    tt("pool", tmpb, li, vrb, ALU.mult)
                    tt("dve", outi, outi, tmpb, ALU.add)
                    ts("dve", outi, outi, -1.0, None, ALU.mult)

            if STOP[0] == 20:
                return
            for qq in range(4):
                gs = slice(4 * qq, 4 * qq + 4)
                g0 = 4 * qq
                cmul(Pr, Pi, 0, bbr, bbi, gs)
                cmul(WSr, WSi, 8, bbr, bbi, gs)
                cmul(Qr, Qi, 16, Cr, Ci, gs, neg_i=True)
                cmul(WXr, WXi, 24, Cr, Ci, gs, neg_i=True)
                if STOP[0] == 21:
                    return
                f2 = lambda a_: a_.rearrange("p g s j -> p g (s j)")
                Pr2, Pi2, WSr2, WSi2, Qr2, Qi2, WXr2, WXi2 = [f2(a_) for a_ in big[:8]]
                bF, bB = ps[0 + (qq % 2) * 2], ps[1 + (qq % 2) * 2]
                for gi_ in range(4):
                    cs_ = slice(gi_ * 128, (gi_ + 1) * 128)
                    mm(bF[:, cs_], Pr2[0:64, gi_, :], Qr2[0:64, gi_, :], start=True, stop=False, sig=False)
                    mm(bF[:, cs_], Pi2[0:64, gi_, :], Qi2[0:64, gi_, :], start=False, stop=True, sig=(gi_ == 3),
                       skip_group_check=True)
                for gi_ in range(4):
                    cs_ = slice(gi_ * 128, (gi_ + 1) * 128)
                    mm(bB[:, cs_], Pr2[64:128, gi_, :], Qr2[64:128, gi_, :], start=True, stop=False, sig=False)
                    mm(bB[:, cs_], Pi2[64:128, gi_, :], Qi2[64:128, gi_, :], start=False, stop=True, sig=(gi_ == 3),
                       skip_group_check=True)
                tmpM = tmpb.rearrange("p g s j -> p g (s j)")
                tt("dve", tmpM, bF.rearrange("p (g m) -> p g m", g=4), MFb, ALU.mult)
                tt("dve", Pr2, bB.rearrange("p (g m) -> p g m", g=4), MBb, ALU.mult)
                tt("dve", tmpM, tmpM, Pr2, ALU.add)
                for gi_ in range(4):
                    stt("dve", M_sb[:, g0 + gi_, :], ident_f, sdp[:, g0 + gi_:g0 + gi_ + 1], tmpM[:, gi_, :],
                        ALU.mult, ALU.add)
                if STOP[0] == 22:
                    return
                for ri, arr in enumerate((WSr2, WSi2)):
                    bank = ps[4 + ri]
                    for gi_ in range(4):
                        tr(bank[:, gi_ * 128:(gi_ + 1) * 128], arr[:, gi_, :], ident_f)
                    cp("act", WS_sb[:, g0:g0 + 4, ri, :], bank.rearrange("p (g m) -> p g m", g=4))
                cp("act", WX_sb[:, gs, 0, :], WXr2)
                cp("act", WX_sb[:, gs, 1, :], WXi2)

            if STOP[0] == 23:
                return
            o = o_run
            U = V(o, [128, 16, 256], BF16); o += 8192
            Y8 = V(O_YT + 6 * 4096, [128, 2, 8, 256], BF16)
            Z8g = V(o, [128, 2, 16, 8, 16], BF16)
            NB = 4
            arrs = [V(o + i * NB * 1024, [128, NB, 256]) for i in range(6)]; o += 6 * NB * 1024
            cs_t, sn_t, Wr, Wi, Wr2, Wi2 = arrs
            Xr = V(o, [128, NB, 258], BF16); o += NB * 516
            Xi = V(o, [128, NB, 258], BF16); o += NB * 516
            Yg = V(o, [128, 256], BF16); o += 512
            g1 = V(o, [128, 256]); o += 1024
            assert o <= RING0, o
            wss = wCD[:, :, 512:768]
            ptr_bf = ps[7].bitcast(BF16)
            for cb in range(2):
                for s in range(8):
                    bank = ps[4 + (cb * 8 + s) % 3]
                    c0 = 1024 * cb + s
                    for k in range(8):
                        mm(bank[:, 0:256], hT[:, k, c0:c0 + 1017:8], wss[:, k, :], start=(k == 0), stop=(k == 7))
                    cp("act", Z8g[:, cb, :, s, :], bank[:, 0:256].rearrange("p (g j) -> p g j", g=16))
            for g4 in range(4):
                for gi_ in range(4):
                    g = g4 * 4 + gi_
                    for cb in range(2):
                        tr(ptr_bf[:, gi_ * 256 + cb * 128: gi_ * 256 + (cb + 1) * 128],
                           Z8g[:, cb, g, :, :].rearrange("p s j -> p (s j)"), ident_bf)
                cp("dve", U[:, g4 * 4:(g4 + 1) * 4, :], ptr_bf.rearrange("p (g c) -> p g c", g=4))
            if STOP[0] == 24:
                return
            memset("dve", Xr[:, :, 0:1], 0.0)
            memset("dve", Xi[:, :, 0:1], 0.0)
            memset("dve", Xr[:, :, 255:257], 0.0)
            memset("dve", Xi[:, :, 255:257], 0.0)
            cidx = consts[:, C_CIDX:C_CIDX + 256].unsqueeze(1).broadcast_to([128, NB, 256])
            for gb in range(16 // NB):
                gsl = slice(gb * NB, gb * NB + NB)
                for gi_ in range(NB):
                    g = gb * NB + gi_
                    mm(psA[:, gi_ * 256:(gi_ + 1) * 256], WS_sb[:, g, 0, :], U[:, g, :], sig=(gi_ % 2 == 1))
                for gi_ in range(NB):
                    g = gb * NB + gi_
                    mm(psB[:, gi_ * 256:(gi_ + 1) * 256], WS_sb[:, g, 1, :], U[:, g, :], sig=(gi_ % 2 == 1))
                tt("dve", Wr2, cidx, th8[:, gsl].unsqueeze(2).broadcast_to([128, NB, 256]), ALU.mult)
                sincos(Wr2, sn_t, cs_t, Wr, Wi)
                for d_ in range(2):
                    pp = slice(64 * d_, 64 * d_ + 64)
                    sr = psA[pp, :].rearrange("p (g c) -> p g c", g=NB)
                    si = psB[pp, :].rearrange("p (g c) -> p g c", g=NB)
                    if d_ == 1:
                        sr = sr[:, :, ::-1]
                        si = si[:, :, ::-1]
                    c_ = cs_t[pp]
                    s_ = sn_t[pp]
                    a_, b_ = Wr2[pp], Wi2[pp]
                    tt("dve", a_, sr, c_, ALU.mult)
                    tt("dve", b_, si, s_, ALU.mult)
                    tt("pool", Wr[pp], a_, b_, ALU.add)
                    tt("dve", a_, si, c_, ALU.mult)
                    tt("dve", b_, sr, s_, ALU.mult)
                    tt("pool", Wi[pp], a_, b_, ALU.subtract)
                if STOP[0] == 25:
                    return
                for gi_ in range(NB):
                    g = gb * NB + gi_
                    r8b = r8[:, g:g + 1].broadcast_to([128, 256])
                    for W_, W2_ in ((Wr, Wr2), (Wi, Wi2)):
                        S.op("dve", lambda W_=W_, W2_=W2_, gi_=gi_, r8b=r8b: nc.vector.tensor_tensor_scan(
                            W2_[:, gi_, :], r8b, W_[:, gi_, :], 0.0, ALU.mult, ALU.add),
                            reads=[r8b, W_[:, gi_, :]], writes=[W2_[:, gi_, :]])
                if STOP[0] == 26:
                    return
                for d_ in range(2):
                    pp = slice(64 * d_, 64 * d_ + 64)
                    a_, b_ = Wr[pp], Wi[pp]
                    if d_ == 0:
                        oxr, oxi = Xr[pp, :, 1:257], Xi[pp, :, 1:257]
                        rd_ = lambda t_: t_
                    else:
                        oxr, oxi = Xr[pp, :, 0:255], Xi[pp, :, 0:255]
                        rd_ = lambda t_: t_[:, :, 254::-1]
                    tt("dve", a_, Wr2[pp], cs_t[pp], ALU.mult)
                    tt("pool", b_, Wi2[pp], sn_t[pp], ALU.mult)
                    tt("dve", oxr, rd_(a_), rd_(b_), ALU.subtract)
                    tt("dve", a_, Wr2[pp], sn_t[pp], ALU.mult)
                    tt("pool", b_, Wi2[pp], cs_t[pp], ALU.mult)
                    tt("dve", oxi, rd_(a_), rd_(b_), ALU.add)
                if STOP[0] == 31:
                    return
                for gi_ in range(NB):
                    g = gb * NB + gi_
                    bank = ps[4 + gi_ % 2][:, 0:256]
                    mm(bank, M_sb[:, g, :], U[:, g, :], start=True, stop=False, sig=False)
                    mm(bank, WX_sb[:, g, 0, :], Xr[:, gi_, 0:256], start=False, stop=False, sig=False)
                    mm(bank, WX_sb[:, g, 1, :], Xi[:, gi_, 0:256], start=False, stop=True)
                    if STOP[0] == 32:
                        return
                    act(g1, bank, AF.Square)
                    ts("dve", g1, g1, 0.044715, 1.0, ALU.mult, ALU.add)
                    tt("dve", g1, g1, bank, ALU.mult)
                    act(g1, g1, AF.Sigmoid, scale=1.5957691216057308)
                    tt("dve", Yg, g1, bank, ALU.mult)
                    if STOP[0] == 33:
                        return
                    for cb in range(2):
                        tr(ptr_bf[:, 512 + cb * 128:512 + (cb + 1) * 128], Yg[:, cb * 128:(cb + 1) * 128], ident_bf)
                    if STOP[0] == 34:
                        return
                    for cb in range(2):
                        cp("dve", Y8[:, cb, :, 16 * g:16 * g + 16],
                           ptr_bf[:, 512 + cb * 128:512 + (cb + 1) * 128].rearrange("p (s i) -> p s i", s=8))
            if STOP[0] == 27:
                return
            ysT = V(o_run, [128, 2, L], BF16)
            for cb in range(2):
                for m in range(2):
                    for s in range(8):
                        tr(ptr_bf[:, s * 128:(s + 1) * 128], Y8[:, cb, s, m * 128:(m + 1) * 128], ident_bf)
                    cp("dve", ysT[:, m, 1024 * cb:1024 * (cb + 1)].rearrange("p (c s) -> p s c", s=8),
                       ptr_bf.rearrange("p (s c) -> p s c", s=8))
            if STOP[0] == 28:
                return
            wgl = V(O_SCR, [128, 2, 256], BF16)
            sgb = [V(O_SCR + 1024 + i * 2048, [128, 512]) for i in range(2)]
            tmp = O_SCR + 5120
            assert tmp + 8192 <= o_run
            yss = V(o_run + 16384, [128, 2, L])
            assert o_run + 32768 <= SCR_END
            S.dma("pool", wgl, wglu_d[l].rearrange("(k p) n -> p k n", p=128))
            for m in range(2):
                for tb in range(4):
                    sl = slice(tb * 512, (tb + 1) * 512)
                    bank = ps[(m * 4 + tb) % 4]
                    for k in range(2):
                        mm(bank, wgl[:, k, m * 128:(m + 1) * 128], ysT[:, k, sl], start=(k == 0), stop=(k == 1))
                    sg = sgb[tb % 2]
                    act(sg, bank, AF.Sigmoid, bias=pvs[:, l, PV_BGLU + m:PV_BGLU + m + 1])
                    tt("dve", yss[:, m, sl], ysT[:, m, sl], sg, ALU.mult)
            gnorm_fm(yss, 3, l, tmp)


        def gu_unit(wg, wu, nf, aT, sgb):
            bi = 0
            for f in range(nf):
                for tb in range(4):
                    sl = slice(tb * 512, (tb + 1) * 512)
                    bg = ps[bi % 2]
                    bu = ps[2 + bi % 2]
                    bi += 1
                    for k in range(8):
                        mm(bg, wg[:, k, f * 128:(f + 1) * 128], hT[:, k, sl], start=(k == 0), stop=(k == 7))
                    sg = sgb[bi % 2]
                    act(sg, bg, AF.Silu)
                    for k in range(8):
                        mm(bu, wu[:, k, f * 128:(f + 1) * 128], hT[:, k, sl], start=(k == 0), stop=(k == 7))
                    tt("dve", aT[:, f, sl], bu, sg, ALU.mult)

        def ffn_dense(l):
            norm_fm(PV_N2, l, O_SCR + 49152)
            aTs = [V(O_YT + i * 16384, [128, 4, L], BF16) for i in range(2)]
            rg = WRing(O_SCR, 2, [128, 8, 512])
            ru = WRing(O_SCR + 16384, 2, [128, 8, 512])
            rd = WRing(O_SCR + 32768, 2, [128, 4, 1024])
            sgb = [V(O_SCR + 49152 + i * 1024, [128, 512], BF16) for i in range(2)]
            assert O_SCR + 49152 + 8192 <= RING0
            fgv = fg_d.rearrange("(k p) n -> p k n", p=128)
            fuv = fu_d.rearrange("(k p) n -> p k n", p=128)
            fdv = fd_d.rearrange("(f p) n -> p f n", p=128)
            units = [(i * 4, 4) for i in range(5)] + [(20, 2)]
            nu = len(units)
            bufs = {}

            def ld_gu(u):
                f0, nf = units[u]
                wg = rg.load(fgv[:, :, f0 * 128:(f0 + nf) * 128], sub=lambda b: b[:, :, 0:nf * 128])
                wu = ru.load(fuv[:, :, f0 * 128:(f0 + nf) * 128], sub=lambda b: b[:, :, 0:nf * 128])
                bufs[("gu", u)] = (wg, wu)

            def ld_d(u):
                f0, nf = units[u]
                bufs[("d", u)] = rd.load(fdv[:, f0:f0 + nf, :], sub=lambda b: b[:, 0:nf, :])

            def down(u):
                f0, nf = units[u]
                wd = bufs.pop(("d", u))
                aT = aTs[u % 2]
                bi = 0
                for m in range(8):
                    for tb in range(4):
                        sl = slice(tb * 512, (tb + 1) * 512)
                        bank = ps[4 + bi % 3]
                        bi += 1
                        for f in range(nf):
                            mm(bank, wd[:, f, m * 128:(m + 1) * 128], aT[:, f, sl], start=(f == 0), stop=(f == nf - 1))
                        tt("dve" if bi % 2 == 0 else "dve", xT[:, m, sl], xT[:, m, sl], bank, ALU.add)

            ld_gu(0); ld_gu(1); ld_d(0); ld_d(1)
            for u in range(nu):
                wg, wu = bufs.pop(("gu", u))
                gu_unit(wg, wu, units[u][1], aTs[u % 2], sgb)
                if u + 2 < nu:
                    ld_gu(u + 2)
                if u >= 1:
                    down(u - 1)
                    if u + 1 < nu:
                        ld_d(u + 1)
            down(nu - 1)

        def moe_and_final(l):
            norm_fm(PV_N2, l, O_SCR + 49152)
            O_XTOK = O_YT
            x_tok = V(O_XTOK, [128, 16, D])
            o = O_XTOK + 65536
            rd = WRing(o, 2, [128, 4, 1024]); o += 16384
            sgb = [V(o + i * 1024, [128, 512], BF16) for i in range(2)]; o += 2048
            cw = V(o, [128, 16, 8]); o += 512
            lg = V(o, [128, 16, 8]); o += 512
            lg2 = V(o, [128, 16, 8]); o += 512
            eq1 = V(o, [128, 16, 8]); o += 512
            eq2 = V(o, [128, 16, 8]); o += 512
            wr_sb = V(o, [128, 8, 8]); o += 256
            wrg = V(o, [128, 8, 8]); o += 256
            sm_ = [V(o + i * 64, [128, 16]) for i in range(8)]; o += 512
            ssq, rstd_t, m1, m2, dd, ee, g1_, g2_ = sm_
            gfin = V(o, [128, D]); o += 4096
            junk = V(o, [128, D]); o += 4096
            assert o <= SCR_END, o
            S.dma("sp", wr_sb, wr_d)
            S.dma("sp", gfin, gfin_d)
            for t in range(16):
                for hf in range(2):
                    bank = ps[(t * 2 + hf) % 4]
                    for kk in range(4):
                        k = hf * 4 + kk
                        tr(bank[:, kk * 128:(kk + 1) * 128], xT[:, k, t * 128:(t + 1) * 128], ident_f)
                    cp("act" if hf == 0 else "dve", x_tok[:, t, hf * 512:(hf + 1) * 512], bank)
            for t in range(16):
                act(junk, x_tok[:, t, :], AF.Square, accum=ssq[:, t:t + 1])
            act(rstd_t, ssq, AF.Sqrt, bias=eps_col, scale=1.0 / D)
            recip(rstd_t, rstd_t)
            for k in range(8):
                ts("dve", wrg[:, k, :], wr_sb[:, k, :], pvs[:, l, PV_N2 + k:PV_N2 + k + 1], None, ALU.mult)
            lb = ps[7]
            for t in range(16):
                for k in range(8):
                    mm(lb[:, t * 8:(t + 1) * 8], xT[:, k, t * 128:(t + 1) * 128], wrg[:, k, :],
                       start=(k == 0), stop=(k == 7), sig=(k == 7 and t == 15), skip_group_check=True)
            b3 = lambda a_: a_.unsqueeze(2).broadcast_to([128, 16, 8])
            tt("dve", lg, lb[:, 0:128].rearrange("p (t e) -> p t e", e=8), b3(rstd_t), ALU.mult)
            S.op("dve", lambda: nc.vector.tensor_reduce(m1, lg, AX.X, ALU.max), reads=[lg], writes=[m1])
            tt("dve", eq1, lg, b3(m1), ALU.is_equal)
            stt("dve", lg2, eq1, -1e30, lg, ALU.mult, ALU.add)
            S.op("dve", lambda: nc.vector.tensor_reduce(m2, lg2, AX.X, ALU.max), reads=[lg2], writes=[m2])
            tt("dve", eq2, lg2, b3(m2), ALU.is_equal)
            tt("dve", dd, m2, m1, ALU.subtract)
            act(ee, dd, AF.Exp)
            ts("dve", g1_, ee, 1.0, None, ALU.add)
            recip(g1_, g1_)
            tt("dve", g2_, ee, g1_, ALU.mult)
            tt("dve", eq1, eq1, b3(g1_), ALU.mult)
            tt("dve", eq2, eq2, b3(g2_), ALU.mult)
            tt("dve", cw, eq1, eq2, ALU.add)
            aTs = [V(O_XT + i * 16384, [128, 4, L], BF16) for i in range(2)]
            rg = WRing(O_XT + 32768, 2, [128, 8, 512])
            ru = WRing(O_XT + 49152, 2, [128, 8, 512])
            NU = 7
            units = [(e, u) for e in range(NE) for u in range(NU)]
            bufs = {}

            def ld_gu(n):
                e, u = units[n]
                wg = rg.load(mg_d[e].rearrange("(k p) n -> p k n", p=128)[:, :, u * 512:(u + 1) * 512])
                wu = ru.load(mu_d[e].rearrange("(k p) n -> p k n", p=128)[:, :, u * 512:(u + 1) * 512])
                bufs[("gu", n)] = (wg, wu)

            def ld_d(n):
                e, u = units[n]
                bufs[("d", n)] = rd.load(md_d[e].rearrange("(f p) n -> p f n", p=128)[:, u * 4:(u + 1) * 4, :])

            def down(n):
                e, u = units[n]
                wd = bufs.pop(("d", n))
                aT = aTs[n % 2]
                bi = 0
                for t in range(16):
                    for hf in range(2):
                        bank = ps[4 + bi % 3]
                        bi += 1
                        for f in range(4):
                            mm(bank, aT[:, f, t * 128:(t + 1) * 128], wd[:, f, hf * 512:(hf + 1) * 512],
                               start=(f == 0), stop=(f == 3))
                        xs = x_tok[:, t, hf * 512:(hf + 1) * 512]
                        stt("dve", xs, bank, cw[:, t, e:e + 1], xs, ALU.mult, ALU.add)

            nun = len(units)
            ld_gu(0); ld_gu(1); ld_d(0); ld_d(1)
            for n in range(nun):
                wg, wu = bufs.pop(("gu", n))
                gu_unit(wg, wu, 4, aTs[n % 2], sgb)
                if n + 2 < nun:
                    ld_gu(n + 2)
                if n >= 1:
                    down(n - 1)
                    if n + 1 < nun:
                        ld_d(n + 1)
            down(nun - 1)
            for t in range(16):
                act(junk, x_tok[:, t, :], AF.Square, accum=ssq[:, t:t + 1])
            act(rstd_t, ssq, AF.Sqrt, bias=eps_col, scale=1.0 / D)
            recip(rstd_t, rstd_t)
            for t in range(16):
                stt("dve", x_tok[:, t, :], x_tok[:, t, :], rstd_t[:, t:t + 1], gfin, ALU.mult, ALU.mult)
                S.dma("sp", out_d[t * 128:(t + 1) * 128, :], x_tok[:, t, :])

        stop_after = STOP[0]
        winvs = [win_d[l].rearrange("(k p) n -> p k n", p=128) for l in range(2)]
        wA = ring_load(0, winvs[0][:, :, 0:768])
        for l in range(nlayers):
            winv = winvs[l]
            woutv = wout_d[l].rearrange("(k p) n -> p k n", p=128)
            with nc.named_scope(f"l{l}_norm1"):
                norm_fm(PV_N1, l, O_SCR + 16384)
            if dbg:
                dump(hT, 8)
            if stop_after == 0:
                break
            with nc.named_scope(f"l{l}_attn"):
                wB = ring_load(1, winv[:, :, 768:1536])
                mixer_attention(l, wA)
            if stop_after in (1, 10, 11, 12, 13, 14):
                dump(yT, 8); break
            with nc.named_scope(f"l{l}_sconv"):
                wCD = ring_load(0, winv[:, :, 1536:2304])
                mixer_sconv(l, wB)
            if stop_after == 2:
                dump(yT, 8); break
            with nc.named_scope(f"l{l}_conf"):
                mixer_conformer(l, wCD)
            if stop_after == 3:
                dump(yT, 8); break
            with nc.named_scope(f"l{l}_s5"):
                mixer_s5(l, wCD)
            if stop_after == 4 or 20 <= stop_after < 40:
                dump(yT, 8); break
            if dbg:
                dump(yT, 8)

            def add_res(m, tb, bank):
                sl = slice(tb * 512, (tb + 1) * 512)
                tt("dve", xT[:, m, sl], xT[:, m, sl], bank, ALU.add)
            with nc.named_scope(f"l{l}_wout"):
                w0 = ring_load(1, woutv[:, :, 0:512])
                w1 = ring_load(0, woutv[:, :, 512:1024])
                proj_w(w0, 0, 512, yT, 8, add_res, ps[0:4])
                proj_w(w1, 0, 512, yT, 8, lambda m, tb, bank: add_res(m + 4, tb, bank), ps[0:4])
            if dbg:
                dump(xT, 8)
            if l % 2 == 0:
                with nc.named_scope(f"l{l}_ffn"):
                    if l + 1 < nlayers:
                        wA = ring_load(0, winvs[l + 1][:, :, 0:768])
                    ffn_dense(l)
                if dbg:
                    dump(xT, 8)
                if nlayers == 1:
                    for k in range(8):
                        S.dma("sp", out_d.rearrange("(k p) d -> p k d", p=128)[:, k, :], xT[:, k, 0:1024])
            else:
                with nc.named_scope(f"l{l}_moe"):
                    moe_and_final(l)
        S.wait_all_dma("sp")
        print(f"[build] inst={S.n_inst} waits={S.n_wait} sems={S.nsem}", flush=True)
    return nc


_CACHE = {}


def kernel(**inputs):
    sh, per_core = host_prep(inputs)
    if "nc" not in _CACHE:
        _CACHE["nc"] = build_program(2, False)
    nc = _CACHE["nc"]
    in_maps = []
    for c in range(NCORES):
        m = dict(sh)
        m["xT"] = per_core[c]
        in_maps.append(m)
    res = run_bass_kernel_spmd(nc, in_maps, core_ids=list(range(NCORES)))
    out = np.stack([np.asarray(res.results[c]["out"], np.float32) for c in range(NCORES)], 0)
    return out
```

```python
import math
import contextlib
import numpy as np
import concourse.bass as bass
import concourse.mybir as mybir
from concourse.bass_utils import run_bass_kernel_spmd

F32 = mybir.dt.float32
BF16 = mybir.dt.bfloat16
I32 = mybir.dt.int32
ALU = mybir.AluOpType
AF = mybir.ActivationFunctionType
AX = mybir.AxisListType

_DT_SIZE = {}


def dt_size(dt):
    s = str(dt)
    if "64" in s:
        return 8
    if "32" in s:
        return 4
    if "16" in s:
        return 2
    return 1


class Sync:
    EPOCH = 24000
    DMA_RING = 6

    def __init__(self, nc, stack):
        self.nc = nc
        self.stack = stack
        self.eng = dict(pe=nc.tensor, act=nc.scalar, dve=nc.vector, pool=nc.gpsimd, sp=nc.sync)
        self.sem = {}
        self.cnt = {}
        self.pend = {}
        self.nsem = 0
        self.waited = {e: {} for e in self.eng}
        self.wr = {}
        self.rd = {}
        self.semobj = {}
        for e in ("pe", "act", "dve", "pool"):
            self._new_sem(e)
        self.ring = {}
        self.ring_pos = {}
        for q in ("sp", "pool", "act"):
            self.ring[q] = []
            self.ring_pos[q] = 0
        self.n_inst = 0
        self.n_wait = 0

    def _alloc_sem(self, name):
        s = self.stack.enter_context(self.nc.semaphore(f"{name}_{self.nsem}"))
        self.nsem += 1
        self.semobj[id(s)] = s
        return s

    def _new_sem(self, e):
        self.sem[e] = self._alloc_sem("s_" + e)
        self.cnt[e] = 0
        self.pend[e] = False

    @staticmethod
    def region(ap):
        t = ap.tensor
        dims = ap.ap
        pstep, pcount = dims[0]
        off = int(ap.offset)
        if pstep > 0:
            p0 = off // pstep
            foff = off - p0 * pstep
        else:
            p0 = 0
            foff = off
        lo = hi = foff
        for st, c in dims[1:]:
            if c <= 0:
                continue
            d = st * (c - 1)
            if d < 0:
                lo += d
            else:
                hi += d
        sz = dt_size(ap.dtype)
        if t.name.startswith("ps"):
            blo = (lo * sz) // 2048 * 2048
            bhi = ((hi + 1) * sz + 2047) // 2048 * 2048
            return (t.name, 0, 128, blo, bhi)
        return (t.name, p0, p0 + pcount, lo * sz, (hi + 1) * sz)

    @staticmethod
    def _ov(a, r):
        return a[0] < r[2] and r[1] < a[1] and a[2] < r[4] and r[3] < a[3]

    @staticmethod
    def _contained(a, r):
        return a[0] >= r[1] and a[1] <= r[2] and a[2] >= r[3] and a[3] <= r[4]

    def _need(self, e, sem, val, deps):
        k = id(sem)
        if self.waited[e].get(k, 0) >= val:
            return
        if deps.get(k, (None, 0))[1] < val:
            deps[k] = (sem, val)

    def _collect(self, e, reads, writes, skip_sem=None):
        deps = {}
        for ap in reads:
            r = self.region(ap)
            for a in self.wr.get(r[0], ()):
                if self._ov(a, r) and a[4] is not skip_sem:
                    self._need(e, a[4], a[5], deps)
        for ap in writes:
            r = self.region(ap)
            for a in self.wr.get(r[0], ()):
                if self._ov(a, r) and a[4] is not skip_sem:
                    self._need(e, a[4], a[5], deps)
            for a in self.rd.get(r[0], ()):
                if self._ov(a, r) and a[4] is not skip_sem:
                    self._need(e, a[4], a[5], deps)
        return deps

    def _emit_waits(self, e, deps):
        for k, (sem, val) in deps.items():
            for en, s in self.sem.items():
                if s is sem and val > self.cnt[en]:
                    raise RuntimeError(f"wait on unsignaled work of {en}: {val} > {self.cnt[en]}")
            self.eng[e].wait_ge(sem, val)
            self.waited[e][k] = val
            self.n_wait += 1

    def _record(self, reads, writes, sem, val):
        for ap in reads:
            r = self.region(ap)
            lst = self.rd.setdefault(r[0], [])
            lst[:] = [a for a in lst if not (a[4] is sem and self._contained(a, r))]
            lst.append((r[1], r[2], r[3], r[4], sem, val))
        for ap in writes:
            r = self.region(ap)
            lst = self.wr.setdefault(r[0], [])
            lst[:] = [a for a in lst if not self._contained(a, r)]
            lst.append((r[1], r[2], r[3], r[4], sem, val))
            lst2 = self.rd.get(r[0])
            if lst2:
                lst2[:] = [a for a in lst2 if not self._contained(a, r)]

    def op(self, e, fn, reads=(), writes=(), signal=True):
        reads = [a for a in reads if a is not None and not isinstance(a, (int, float))]
        skip = self.sem[e] if e == "pe" else None
        deps = self._collect(e, reads, writes, skip_sem=skip)
        self._emit_waits(e, deps)
        ins = fn()
        self.n_inst += 1
        if signal:
            if self.cnt[e] >= self.EPOCH and not self.pend[e]:
                old = self.sem[e]
                oldc = self.cnt[e]
                self._new_sem(e)
            self.cnt[e] += 1
            ins.then_inc(self.sem[e], 1)
            self.pend[e] = False
            self._record(reads, writes, self.sem[e], self.cnt[e])
        else:
            self.pend[e] = True
            self._record(reads, writes, self.sem[e], self.cnt[e] + 1)
        return ins

    def dma(self, q, out, in_, **kw):
        reads = [in_] if str(in_.space) != "DRAM" and "DRAM" not in str(in_.space).upper() else []
        writes = [out] if "DRAM" not in str(out.space).upper() else []
        ring = self.ring[q]
        pos = self.ring_pos[q]
        if len(ring) < self.DMA_RING:
            ring.append([self._alloc_sem("d_" + q), 0])
        slot = ring[pos % self.DMA_RING]
        self.ring_pos[q] = pos + 1
        deps = self._collect(q, reads, writes)
        if slot[1] > 0:
            self._need(q, slot[0], slot[1], deps)
        if slot[1] >= self.EPOCH * 2:
            self._emit_waits(q, deps)
            deps = {}
            slot[0] = self._alloc_sem("d_" + q)
            slot[1] = 0
        self._emit_waits(q, deps)
        ins = self.eng[q].dma_start(out=out, in_=in_, **kw)
        slot[1] += 16
        ins.then_inc(slot[0], 16)
        self.n_inst += 1
        self._record(reads, writes, slot[0], slot[1])
        return ins

    def wait_all_dma(self, e="sp"):
        for q, ring in self.ring.items():
            for sem, val in ring:
                if val > 0 and self.waited[e].get(id(sem), 0) < val:
                    self.eng[e].wait_ge(sem, val)
                    self.waited[e][id(sem)] = val


D = 1024
L = 2048
NCORES = 8
GRID_W = 64
ROWS = 32
NEG = -30000.0
FF_D = 2816
FF_E = 3584
NE = 8
EPS = 1e-6

PV_N1 = 0
PV_N2 = 8
PV_GN = 16
PV_SCW = 24
PV_CFW = 30
PV_CFB = 92
PV_LNG = 94
PV_LNB = 96
PV_BGLU = 98
PV_COLS = 100

C_ONES = 0
C_ID = 128
C_MF = 256
C_MB = 384
C_KK = 512
C_CIDX = 545
C_COLS = 801


def _row_start(r):
    return int(np.clip(r - 4, 0, ROWS - 8))


def na_plan():
    plans = []
    keys = {}
    for i in range(16):
        r0 = 2 * i
        lo = min(_row_start(r0), _row_start(r0 + 1))
        hi = max(_row_start(r0), _row_start(r0 + 1)) + 7
        a0, a1 = lo // 2, hi // 2
        lst = []
        for a in range(a0, a1 + 1):
            key = (2 * a - r0, _row_start(r0) - r0, _row_start(r0 + 1) - r0)
            if key not in keys:
                keys[key] = len(keys)
            lst.append((a, keys[key]))
        plans.append(lst)
    return plans, keys


def na_tables(rpb):
    plans, keys = na_plan()
    out = np.empty((len(keys), 128, 4, 128), np.float32)
    kl = np.arange(128)
    kr_l, kc = kl // 64, kl % 64
    ql = np.arange(128)
    qr_l, qc = ql // 64, ql % 64
    qcs = np.clip(qc - 8, 0, GRID_W - 16)
    for (da, rs0, rs1), idx in keys.items():
        krow = (da + kr_l)[:, None]
        qrow = qr_l[None, :]
        rs = np.where(qr_l == 0, rs0, rs1)[None, :]
        row_ok = (krow >= rs) & (krow < rs + 8)
        col_ok = (kc[:, None] >= qcs[None, :]) & (kc[:, None] < qcs[None, :] + 16)
        dr = np.clip(krow - qrow + 7, 0, 14)
        dc = np.clip(kc[:, None] - qc[None, :] + 15, 0, 30)
        ok = row_ok & col_ok
        for h in range(4):
            out[idx, :, h, :] = np.where(ok, rpb[h][dr, dc], np.float32(NEG))
    return out.reshape(len(keys), 128, 512)


def fm_cols(v, nchunk):
    return np.ascontiguousarray(np.asarray(v, np.float32).reshape(nchunk, 128).T)


def make_consts():
    c = np.zeros((128, C_COLS), np.float32)
    c[:, C_ONES:C_ONES + 128] = 1.0
    c[:, C_ID:C_ID + 128] = np.eye(128, dtype=np.float32)
    sp = np.arange(128)[:, None] // 16
    s = np.arange(128)[None, :] // 16
    c[:, C_MF:C_MF + 128] = (sp <= s)
    c[:, C_MB:C_MB + 128] = (sp >= s)
    kk = np.zeros((128, 33), np.float32)
    sv = np.arange(8)
    kk[:64, 0:8] = -sv
    kk[64:, 0:8] = sv
    kk[:64, 8:16] = 7 - sv
    kk[64:, 8:16] = sv
    kk[:64, 16:24] = sv
    kk[64:, 16:24] = -sv
    kk[:64, 24:32] = sv + 1
    kk[64:, 24:32] = 8 - sv
    kk[:, 32] = 1
    c[:, C_KK:C_KK + 33] = kk
    c[:, C_CIDX:C_CIDX + 256] = np.arange(256)[None, :]
    return c


def host_prep(inp):
    f = lambda a: np.ascontiguousarray(np.asarray(a, np.float32))
    sh = {}
    pv = np.zeros((2, 128, PV_COLS), np.float32)
    for l in range(2):
        pv[l, :, PV_N1:PV_N1 + 8] = fm_cols(inp["norm1_g"][l], 8)
        pv[l, :, PV_N2:PV_N2 + 8] = fm_cols(inp["norm2_g"][l], 8)
        pv[l, :, PV_GN:PV_GN + 8] = fm_cols(inp["grp_norm_g"][l], 8)
        for t in range(3):
            pv[l, :, PV_SCW + 2 * t:PV_SCW + 2 * t + 2] = fm_cols(inp["sc_conv_w"][l][t], 2)
        for t in range(31):
            pv[l, :, PV_CFW + 2 * t:PV_CFW + 2 * t + 2] = fm_cols(inp["cf_conv_w"][l][t], 2)
        pv[l, :, PV_CFB:PV_CFB + 2] = fm_cols(inp["cf_conv_b"][l], 2)
        pv[l, :, PV_LNG:PV_LNG + 2] = fm_cols(inp["cf_ln_g"][l], 2)
        pv[l, :, PV_LNB:PV_LNB + 2] = fm_cols(inp["cf_ln_b"][l], 2)
        pv[l, :, PV_BGLU:PV_BGLU + 2] = fm_cols(inp["ssm_b_glu"][l], 2)
    sh["pv"] = pv
    sh["consts"] = make_consts()
    sh["natab"] = np.stack([na_tables(np.asarray(inp["na_rpb"][l], np.float32)) for l in range(2)])
    a_re = f(inp["ssm_a_re"])
    a_im = f(inp["ssm_a_im"])
    ldt = f(inp["ssm_log_dt"])
    s5a = np.zeros((2, 128, 16, 3), np.float32)
    s5a[..., 0] = a_re.transpose(0, 1, 3, 2).reshape(2, 128, 16)
    s5a[..., 1] = a_im.transpose(0, 1, 3, 2).reshape(2, 128, 16)
    s5a[..., 2] = np.broadcast_to(ldt[:, :, None, :], (2, 2, 64, 16)).reshape(2, 128, 16)
    sh["s5a"] = s5a
    b_re = f(inp["ssm_b_re"])
    b_im = f(inp["ssm_b_im"])
    sh["s5b"] = np.ascontiguousarray(np.stack([b_re, b_im], 0).transpose(1, 2, 4, 3, 0, 5).reshape(2, 128, 16, 2, 16))
    c_re = f(inp["ssm_c_re"])
    c_im = f(inp["ssm_c_im"])
    sh["s5c"] = np.ascontiguousarray(np.stack([c_re, c_im], 0).transpose(1, 2, 5, 3, 0, 4).reshape(2, 128, 16, 2, 16))
    d = f(inp["ssm_d"])
    dd = d.reshape(2, 16, 16)
    sh["s5d"] = np.ascontiguousarray(np.broadcast_to(dd.transpose(0, 2, 1)[:, None, :, :], (2, 8, 16, 16)).reshape(2, 128, 16))
    sh["wglu"] = f(inp["ssm_w_glu"])
    sh["win"] = f(inp["w_in"])
    sh["wout"] = f(inp["w_out"])
    sh["fg"] = f(inp["ffn_w_gate"][0])
    sh["fu"] = f(inp["ffn_w_up"][0])
    sh["fd"] = f(inp["ffn_w_down"][0])
    sh["mg"] = f(inp["moe_w_gate"][0])
    sh["mu"] = f(inp["moe_w_up"][0])
    sh["md"] = f(inp["moe_w_down"][0])
    sh["wr"] = np.ascontiguousarray(f(inp["moe_w_router"][0]).reshape(8, 128, 8).transpose(1, 0, 2))
    sh["gfin"] = np.ascontiguousarray(np.broadcast_to(f(inp["final_norm_g"])[None, :], (128, 1024)))
    x = f(inp["x"])
    per_core = [np.ascontiguousarray(x[b].T) for b in range(NCORES)]
    return sh, per_core


ARENA_BYTES = 212800
O_XT = 0
O_HT = 65536
O_YT = 98304
O_SCR = 131072
O_CONST = 208000
SCR_END = O_CONST
STOP = [99]


def build_program(nlayers=2, dbg=False):
    nc = bass.Bass("TRN2", target_bir_lowering=False)
    dr = {}

    def din(name, shape):
        dr[name] = nc.dram_tensor(name, list(shape), F32, kind="ExternalInput").ap()
        return dr[name]

    xT_d = din("xT", [D, L])
    win_d = din("win", [2, D, 2304])
    wout_d = din("wout", [2, D, D])
    fg_d = din("fg", [D, FF_D])
    fu_d = din("fu", [D, FF_D])
    fd_d = din("fd", [FF_D, D])
    mg_d = din("mg", [NE, D, FF_E])
    mu_d = din("mu", [NE, D, FF_E])
    md_d = din("md", [NE, FF_E, D])
    wr_d = din("wr", [128, 8, 8])
    pv_d = din("pv", [2, 128, PV_COLS])
    consts_d = din("consts", [128, C_COLS])
    plans, tkeys = na_plan()
    NT = len(tkeys)
    natab_d = din("natab", [2, NT, 128, 512])
    s5a_d = din("s5a", [2, 128, 16, 3])
    s5b_d = din("s5b", [2, 128, 16, 2, 16])
    s5c_d = din("s5c", [2, 128, 16, 2, 16])
    s5d_d = din("s5d", [2, 128, 16])
    wglu_d = din("wglu", [2, 256, 256])
    gfin_d = din("gfin", [128, 1024])
    out_d = nc.dram_tensor("out", [L, D], F32, kind="ExternalOutput").ap()
    dbg_d = None
    if dbg:
        dbg_d = nc.dram_tensor("dbg", [8, 128, 8, 2048], F32, kind="ExternalOutput").ap()

    st = contextlib.ExitStack()
    with st:
        S = Sync(nc, st)
        arena_t = st.enter_context(nc.sbuf_tensor("arena", [128, ARENA_BYTES // 4], F32))
        arena = arena_t[:]
        psA = st.enter_context(nc.psum_tensor("psA", [128, 1024], F32))[:]
        psB = st.enter_context(nc.psum_tensor("psB", [128, 1024], F32))[:]
        ps = [psA[:, 0:512], psA[:, 512:1024], psB[:, 0:512], psB[:, 512:1024]]
        ps += [st.enter_context(nc.psum_tensor(f"ps{i}", [128, 512], F32))[:] for i in range(4, 8)]
        EN = dict(dve=nc.vector, pool=nc.gpsimd)

        def V(off, shape, dt=F32):
            assert off % 4 == 0
            n = 1
            for s_ in shape[1:]:
                n *= s_
            nb = n * dt_size(dt)
            assert nb % 4 == 0
            ap = arena[:, off // 4: off // 4 + nb // 4]
            if dt != F32:
                ap = ap.bitcast(dt)
            if len(shape) == 3:
                ap = ap.rearrange("p (a b) -> p a b", a=shape[1])
            elif len(shape) == 4:
                ap = ap.rearrange("p (a b c) -> p a b c", a=shape[1], b=shape[2])
            elif len(shape) == 5:
                ap = ap.rearrange("p (a b c d) -> p a b c d", a=shape[1], b=shape[2], c=shape[3])
            return ap

        def mm(out, lhsT, rhs, start=True, stop=True, sig=None, **kw):
            if lhsT.partition_size() <= 64 and "tile_position" not in kw:
                kw["tile_position"] = (lhsT.base_partition(), 0)
            S.op("pe", lambda: nc.tensor.matmul(out, lhsT, rhs, start=start, stop=stop, **kw),
                 reads=[lhsT, rhs], writes=[out], signal=(stop if sig is None else sig))

        def tr(out, in_, ident):
            S.op("pe", lambda: nc.tensor.transpose(out, in_, ident), reads=[in_, ident], writes=[out])

        def act(out, in_, func, bias=None, scale=None, accum=None):
            kw = {}
            if bias is not None:
                kw["bias"] = bias
            if scale is not None:
                kw["scale"] = scale
            if accum is not None:
                kw["accum_out"] = accum
            S.op("act", lambda: nc.scalar.activation(out, in_, func, **kw),
                 reads=[in_, bias, scale], writes=[out] + ([accum] if accum is not None else []))

        def tt(e, out, a, b, op):
            S.op(e, lambda: EN[e].tensor_tensor(out, a, b, op), reads=[a, b], writes=[out])

        def ts(e, out, a, s1, s2, op0, op1=None):
            if op1 is None:
                S.op(e, lambda: EN[e].tensor_scalar(out, a, s1, None, op0), reads=[a, s1], writes=[out])
            else:
                S.op(e, lambda: EN[e].tensor_scalar(out, a, s1, s2, op0, op1), reads=[a, s1, s2], writes=[out])

        def stt(e, out, a, s, b, op0, op1):
            e = "dve"
            S.op(e, lambda: EN[e].scalar_tensor_tensor(out, a, s, b, op0, op1), reads=[a, s, b], writes=[out])

        def cp(e, out, a):
            if e == "act":
                S.op("act", lambda: nc.scalar.copy(out, a), reads=[a], writes=[out])
            else:
                S.op(e, lambda: EN[e].tensor_copy(out, a), reads=[a], writes=[out])

        def memset(e, out, val):
            S.op(e, lambda: EN[e].memset(out, val), reads=[], writes=[out])

        def recip(out, a):
            S.op("dve", lambda: nc.vector.reciprocal(out, a), reads=[a], writes=[out])

        dbg_slot = [0]

        def dump(ap_fm, nchunk, cast=False):
            if not dbg:
                return
            i = dbg_slot[0]
            dbg_slot[0] += 1
            for k in range(nchunk):
                S.dma("pool", dbg_d[i, :, k, :], ap_fm[:, k, :])

        xT = V(O_XT, [128, 8, L])
        hT = V(O_HT, [128, 8, L], BF16)
        yT = V(O_YT, [128, 8, L], BF16)
        consts = V(O_CONST, [128, C_COLS])
        pvs = V(O_CONST + 3204, [128, 2, PV_COLS])
        o_misc = O_CONST + 3204 + 800
        ident_bf = V(o_misc, [128, 128], BF16)
        ones_bf = V(o_misc + 256, [128, 128], BF16)
        small = V(o_misc + 512, [128, 64])
        assert o_misc + 768 <= ARENA_BYTES
        ones_f = consts[:, C_ONES:C_ONES + 128]
        ident_f = consts[:, C_ID:C_ID + 128]

        S.dma("sp", consts, consts_d)
        for l in range(2):
            S.dma("sp", pvs[:, l, :], pv_d[l])
        xTv = xT_d.rearrange("(k p) t -> p k t", p=128)
        for k in range(8):
            S.dma("sp", xT[:, k, :], xTv[:, k, :])
        cp("dve", ident_bf, ident_f)
        cp("dve", ones_bf, ones_f)

        def rstd_block(chunks, inv_n, rstd_out, bank, sqa, sqb):
            nk = len(chunks)
            for k, c in enumerate(chunks):
                sq = sqa if k % 2 == 0 else sqb
                act(sq, c, AF.Square)
                mm(bank, ones_f, sq, start=(k == 0), stop=(k == nk - 1), sig=True)
            act(rstd_out, bank, AF.Sqrt, bias=eps_col, scale=inv_n)
            recip(rstd_out, rstd_out)

        eps_col = small[:, 0:1]
        memset("dve", eps_col, EPS)

        def norm_fm(gcol0, l, o_tmp):
            sqa = V(o_tmp, [128, 512])
            sqb = V(o_tmp + 2048, [128, 512])
            for tb in range(4):
                rs = V(o_tmp + 4096 + (tb % 2) * 2048, [128, 512])
                sl = slice(tb * 512, (tb + 1) * 512)
                rstd_block([xT[:, k, sl] for k in range(8)], 1.0 / D, rs, ps[7], sqa, sqb)
                for k in range(8):
                    stt("dve" if k % 2 == 0 else "pool", hT[:, k, sl], xT[:, k, sl],
                        pvs[:, l, gcol0 + k:gcol0 + k + 1], rs, ALU.mult, ALU.mult)

        def gnorm_fm(src, gi, l, o_tmp):
            sqa = V(o_tmp, [128, 512])
            sqb = V(o_tmp + 2048, [128, 512])
            for tb in range(4):
                rs = V(o_tmp + 4096 + (tb % 2) * 2048, [128, 512])
                sl = slice(tb * 512, (tb + 1) * 512)
                rstd_block([src[:, c, sl] for c in range(2)], 1.0 / 256, rs, ps[7], sqa, sqb)
                for c in range(2):
                    gc = PV_GN + 2 * gi + c
                    stt("dve", yT[:, 2 * gi + c, sl], src[:, c, sl], pvs[:, l, gc:gc + 1], rs, ALU.mult, ALU.mult)

        class WRing:
            def __init__(self, off, nbuf, shape):
                n = 1
                for s_ in shape[1:]:
                    n *= s_
                self.bufs = [V(off + i * n * 2, shape, BF16) for i in range(nbuf)]
                self.i = 0
                self.nbytes = nbuf * n * 2

            def load(self, src, sub=None):
                b = self.bufs[self.i % len(self.bufs)]
                self.i += 1
                dst = b if sub is None else sub(b)
                if len(src.shape) == 3 and src.shape[1] > 1 and src.shape[2] * 4 >= 2048:
                    half = src.shape[1] // 2
                    S.dma("pool", dst[:, 0:half, :], src[:, 0:half, :])
                    S.dma("pool", dst[:, half:, :], src[:, half:, :])
                elif len(src.shape) == 3 and src.shape[1] > 1:
                    for k in range(src.shape[1]):
                        S.dma("pool", dst[:, k, :], src[:, k, :])
                else:
                    S.dma("pool", dst, src)
                return b

        def proj_fm(wv, col0, ncols, actT, K, consumer, ring, banks, unit=256):
            nunits = ncols // unit
            loaded = {}

            def ld(u):
                loaded[u] = ring.load(wv[:, :, col0 + u * unit: col0 + (u + 1) * unit])
            ld(0)
            if nunits > 1:
                ld(1)
            bi = 0
            for u in range(nunits):
                if u + 2 < nunits:
                    ld(u + 2)
                wb = loaded.pop(u)
                for mi in range(unit // 128):
                    m = u * (unit // 128) + mi
                    for tb in range(4):
                        bank = banks[bi % len(banks)]
                        bi += 1
                        for k in range(K):
                            mm(bank, wb[:, k, mi * 128:(mi + 1) * 128], actT[:, k, tb * 512:(tb + 1) * 512],
                               start=(k == 0), stop=(k == K - 1))
                        consumer(m, tb, bank)

        def proj_w(wb, c0, ncols, actT, K, consumer, banks):
            bi = 0
            for m in range(ncols // 128):
                for tb in range(4):
                    bank = banks[bi % len(banks)]
                    bi += 1
                    for k in range(K):
                        mm(bank, wb[:, k, c0 + m * 128:c0 + (m + 1) * 128], actT[:, k, tb * 512:(tb + 1) * 512],
                           start=(k == 0), stop=(k == K - 1))
                    consumer(m, tb, bank)

        RING0 = SCR_END - 12288
        RING1 = SCR_END - 24576
        MIX_END = RING1
        ringbuf = [V(RING0, [128, 8, 768], BF16), V(RING1, [128, 8, 768], BF16)]

        def ring_load(slot, src):
            n = src.shape[2]
            dst = ringbuf[slot][:, :, 0:n]
            S.dma("pool", dst[:, 0:4, :], src[:, 0:4, :])
            S.dma("pool", dst[:, 4:8, :], src[:, 4:8, :])
            return ringbuf[slot]


        def mixer_attention(l, wA):
            o = O_SCR
            QT = V(o, [128, 2, L], BF16); o += 8192
            KT = V(o, [128, 2, L], BF16); o += 8192
            Vaug = V(o, [128, 16, 4, 65], BF16); o += 8320
            Etab = V(O_YT + 8192, [128, NT, 512], BF16)
            assert NT * 1024 <= 24576
            Pb = [V(o + i * 1024, [128, 512], BF16) for i in range(2)]; o += 2048
            stg = [V(o, [128, 512])] * 2; o += 2048
            ytoks = [V(o + i * 1024, [128, 4, 64]) for i in range(2)]; o += 2048
            ybfs = [V(o + i * 512, [128, 256], BF16) for i in range(2)]; o += 1024
            junk = stg[0][:, 0:256]
            assert o <= MIX_END, o

            for t in range(NT):
                S.dma("sp", stg[t % 2], natab_d[l, t])
                act(Etab[:, t, :], stg[t % 2], AF.Exp)
            memset("dve", Vaug[:, :, :, 64:65], 1.0)

            if STOP[0] == 10:
                return
            def consumer(m, tb, bank):
                dst = QT if m < 2 else KT
                cp("act", dst[:, m % 2, tb * 512:(tb + 1) * 512], bank)
            proj_w(wA, 0, 512, hT, 8, consumer, ps[0:4])
            if STOP[0] == 11:
                return
            wvb = wA[:, :, 512:768]
            for t in range(16):
                bank = ps[t % 4]
                for k in range(8):
                    mm(bank[:, 0:256], hT[:, k, t * 128:(t + 1) * 128], wvb[:, k, :], start=(k == 0), stop=(k == 7))
                cp("dve", Vaug[:, t, :, 0:64], bank[:, 0:256].rearrange("p (h d) -> p h d", h=4))
            if STOP[0] == 12:
                return
            ptr_bf = ps[7].bitcast(BF16)
            for i in range(16):
                if STOP[0] == 13 and i == 1:
                    return
                pvb = ps[i % 2]
                ytok, ybf = ytoks[i % 2], ybfs[i % 2]
                ss = small[:, 1 + i % 2:2 + i % 2]
                rec4 = small[:, 4:8] if i % 2 == 0 else small[:, 12:16]
                pvv = pvb[:, 0:260].rearrange("p (h d) -> p h d", h=4)
                plan = plans[i]
                POS = {0: 0, 2: 1, 1: 2, 3: 3}
                for ci, (a, ti) in enumerate(plan):
                    sb0, sb1 = (ps[4], ps[5]) if ci % 2 == 0 else (ps[2], ps[3])
                    for h in range(4):
                        hp = (h % 2) * 64
                        sbh = sb0 if hp == 0 else sb1
                        mm(sbh[:, (h // 2) * 128:(h // 2 + 1) * 128], KT[hp:hp + 64, h // 2, a * 128:(a + 1) * 128],
                           QT[hp:hp + 64, h // 2, i * 128:(i + 1) * 128], start=True, stop=True, sig=True)
                    P = Pb[ci % 2]
                    Ev = Etab[:, ti, :].rearrange("p (h q) -> p h q", h=4)
                    act(P[:, 0:256], sb0[:, 0:256], AF.Exp, scale=0.125)
                    act(P[:, 256:512], sb1[:, 0:256], AF.Exp, scale=0.125)
                    tt("dve", P[:, 0:256].rearrange("p (h q) -> p h q", h=2), P[:, 0:256].rearrange("p (h q) -> p h q", h=2),
                       Ev[:, 0::2, :], ALU.mult)
                    tt("dve", P[:, 256:512].rearrange("p (h q) -> p h q", h=2), P[:, 256:512].rearrange("p (h q) -> p h q", h=2),
                       Ev[:, 1::2, :], ALU.mult)
                    for h in range(4):
                        first = (ci == 0 and h == 0)
                        last = (ci == len(plan) - 1 and h == 3)
                        mm(pvv[:, h, :], P[:, POS[h] * 128:(POS[h] + 1) * 128], Vaug[:, a, h, :], start=first, stop=last,
                           sig=(h == 3), skip_group_check=True)
                if STOP[0] == 14:
                    return
                recip(rec4, pvv[:, :, 64])
                tt("dve", ytok, pvv[:, :, 0:64], rec4.unsqueeze(2).broadcast_to([128, 4, 64]), ALU.mult)
                yflat = ytok.rearrange("p h d -> p (h d)")
                act(junk, yflat, AF.Square, accum=ss)
                act(ss, ss, AF.Sqrt, bias=eps_col, scale=1.0 / 256)
                recip(ss, ss)
                ts("dve", ybf, yflat, ss, None, ALU.mult)
                for m in range(2):
                    tr(ptr_bf[:, m * 128:(m + 1) * 128], ybf[:, m * 128:(m + 1) * 128], ident_bf)
                for m in range(2):
                    gc = PV_GN + m
                    ts("dve", yT[:, m, i * 128:(i + 1) * 128], ptr_bf[:, m * 128:(m + 1) * 128],
                       pvs[:, l, gc:gc + 1], None, ALU.mult)

        def mixer_sconv(l, wB):
            o = O_SCR
            scb = V(o, [128, 2, L], BF16); o += 8192
            scv = V(o, [128, 2, L]); o += 16384
            sco = V(o, [128, 2, L]); o += 16384
            tmp = o
            assert o + 8192 <= MIX_END

            def consumer(m, tb, bank):
                mm_ = m - 6
                sl = slice(tb * 512, (tb + 1) * 512)
                if mm_ < 2:
                    cp("act", scb[:, mm_, sl], bank)
                elif mm_ < 4:
                    cp("act", scv[:, mm_ - 2, sl], bank)
                else:
                    tt("dve", scv[:, mm_ - 4, sl], scv[:, mm_ - 4, sl], bank, ALU.mult)
            proj_w(wB, 0, 768, hT, 8, lambda m, tb, bank: consumer(m + 6, tb, bank), ps[0:4])
            for c in range(2):
                w = lambda t: pvs[:, l, PV_SCW + 2 * t + c:PV_SCW + 2 * t + c + 1]
                ts("dve", sco[:, c, :], scv[:, c, :], w(1), None, ALU.mult)
                stt("dve", sco[:, c, 1:L], scv[:, c, 0:L - 1], w(0), sco[:, c, 1:L], ALU.mult, ALU.add)
                stt("dve", sco[:, c, 0:L - 1], scv[:, c, 1:L], w(2), sco[:, c, 0:L - 1], ALU.mult, ALU.add)
                tt("dve", sco[:, c, :], sco[:, c, :], scb[:, c, :], ALU.mult)
            gnorm_fm(sco, 1, l, tmp)

        def mixer_conformer(l, wCD):
            o = O_SCR
            cfa = V(o, [128, 2, L]); o += 16384
            PADW = L + 32
            vpad = V(o, [128, 2, PADW], BF16); o += 2 * PADW * 2
            dg = V(o, [128, 2, 31, 128], BF16); o += 2 * 31 * 256
            sig = [V(o, [128, 512])] * 2; o += 2048
            ycf = cfa
            tmp = o
            assert o + 8192 <= MIX_END, o
            for c in range(2):
                memset("dve", vpad[:, c, 0:15], 0.0)
                memset("dve", vpad[:, c, 15 + L:PADW], 0.0)
                for t in range(31):
                    col = PV_CFW + 2 * t + c
                    act(dg[:, c, t, :], ident_f, AF.Identity, scale=pvs[:, l, col:col + 1])

            def consumer(m, tb, bank):
                sl = slice(tb * 512, (tb + 1) * 512)
                if m < 2:
                    cp("act", cfa[:, m, sl], bank)
                else:
                    sg = sig[tb % 2]
                    act(sg, bank, AF.Sigmoid)
                    tt("dve", vpad[:, m - 2, 15 + tb * 512:15 + (tb + 1) * 512], cfa[:, m - 2, sl], sg, ALU.mult)
            proj_w(wCD, 0, 512, hT, 8, consumer, ps[0:4])
            bi = 0
            for c in range(2):
                for tb in range(4):
                    bank = ps[bi % 4]; bi += 1
                    for t in range(31):
                        mm(bank, dg[:, c, t, :], vpad[:, c, tb * 512 + t: tb * 512 + t + 512], start=(t == 0), stop=(t == 30))
                    act(cfa[:, c, tb * 512:(tb + 1) * 512], bank, AF.Identity, bias=pvs[:, l, PV_CFB + c:PV_CFB + c + 1])
            sqa = V(tmp, [128, 512]); sqb = V(tmp + 2048, [128, 512])
            mean = V(tmp + 4096, [128, 512]); var = V(tmp + 6144, [128, 512])
            for tb in range(4):
                sl = slice(tb * 512, (tb + 1) * 512)
                for c in range(2):
                    mm(ps[6], ones_f, cfa[:, c, sl], start=(c == 0), stop=(c == 1))
                for c in range(2):
                    sq = sqa if c == 0 else sqb
                    act(sq, cfa[:, c, sl], AF.Square)
                    mm(ps[7], ones_f, sq, start=(c == 0), stop=(c == 1))
                ts("dve", mean, ps[6], 1.0 / 256, None, ALU.mult)
                tt("dve", sqa, mean, mean, ALU.mult)
                stt("dve", var, ps[7], 1.0 / 256, sqa, ALU.mult, ALU.subtract)
                act(var, var, AF.Sqrt, bias=eps_col, scale=1.0)
                recip(var, var)
                for c in range(2):
                    tt("dve", sqb, cfa[:, c, sl], mean, ALU.subtract)
                    tt("dve", sqb, sqb, var, ALU.mult)
                    act(ycf[:, c, sl], sqb, AF.Silu, bias=pvs[:, l, PV_LNB + c:PV_LNB + c + 1],
                        scale=pvs[:, l, PV_LNG + c:PV_LNG + c + 1])
            gnorm_fm(ycf, 2, l, tmp)


        PI = math.pi

        def mixer_s5(l, wCD):
            o = O_SCR
            M_sb = V(o, [128, 16, 128], BF16); o += 4096
            WS_sb = V(o, [128, 16, 2, 128], BF16); o += 8192
            WX_sb = V(o, [128, 16, 2, 128], BF16); o += 8192
            r8 = V(o, [128, 16]); o += 64
            th8 = V(o, [128, 16]); o += 64
            o_run = o
            negpi = small[:, 8:9]
            memset("dve", negpi, -PI)

            def sincos(ang, osin, ocos, tmp, tmp2):
                ts("dve", tmp.bitcast(I32), ang, 1.0 / (2 * PI), None, ALU.mult)
                cp("dve", tmp2, tmp.bitcast(I32))
                stt("dve", tmp2, tmp2, -2 * PI, ang, ALU.mult, ALU.add)
                act(tmp, tmp2, AF.Sin, scale=0.5)
                act(ocos, tmp2, AF.Sin, scale=0.25)
                tt("dve", ocos, ocos, ocos, ALU.mult)
                ts("dve", ocos, ocos, -2.0, 1.0, ALU.mult, ALU.add)
                stt("dve", osin, tmp, 2.0, ocos, ALU.mult, ALU.mult)
                tt("dve", ocos, tmp, tmp, ALU.mult)
                ts("dve", ocos, ocos, -2.0, 1.0, ALU.mult, ALU.add)

            sa = V(o, [128, 16, 3]); o += 192
            sbp = V(o, [128, 16, 2, 16]); o += 2048
            scp = V(o, [128, 16, 2, 16]); o += 2048
            sdp = V(o, [128, 16]); o += 64
            S.dma("sp", sa, s5a_d[l])
            S.dma("sp", sbp, s5b_d[l])
            S.dma("sp", scp, s5c_d[l])
            S.dma("sp", sdp, s5d_d[l])
            sm = [V(o + i * 64, [128, 16]) for i in range(10)]; o += 640
            dtv, arv, thv, nr, den, gr, gi, t0, t1, li1 = sm
            a_re = sa[:, :, 0]
            a_im = sa[:, :, 1]
            act(dtv, sa[:, :, 2], AF.Exp)
            tt("dve", arv, a_re, dtv, ALU.mult)
            tt("dve", thv, a_im, dtv, ALU.mult)
            ts("dve", t0, arv, 8.0, None, ALU.mult)
            act(r8, t0, AF.Exp)
            ts("dve", th8, thv, 8.0, None, ALU.mult)
            NS = 33
            ark = V(o, [128, 16, NS]); o += 16 * NS * 4
            thk = V(o, [128, 16, NS]); o += 16 * NS * 4
            LR = V(o, [128, 16, NS]); o += 16 * NS * 4
            LI = V(o, [128, 16, NS]); o += 16 * NS * 4
            tk = V(o, [128, 16, NS]); o += 16 * NS * 4
            tk2 = V(o, [128, 16, NS]); o += 16 * NS * 4
            KKb = consts[:, C_KK:C_KK + NS].unsqueeze(1).broadcast_to([128, 16, NS])
            tt("dve", ark, KKb, arv.unsqueeze(2).broadcast_to([128, 16, NS]), ALU.mult)
            tt("dve", thk, KKb, thv.unsqueeze(2).broadcast_to([128, 16, NS]), ALU.mult)
            act(ark, ark, AF.Exp)
            sincos(thk, LI, LR, tk, tk2)
            tt("dve", LR, LR, ark, ALU.mult)
            tt("dve", LI, LI, ark, ALU.mult)
            ts("dve", nr, LR[:, :, 32], -1.0, None, ALU.add)
            cp("dve", li1, LI[:, :, 32])
            tt("dve", den, a_re, a_re, ALU.mult)
            tt("dve", t0, a_im, a_im, ALU.mult)
            tt("dve", den, den, t0, ALU.add)
            recip(den, den)
            tt("dve", gr, nr, a_re, ALU.mult)
            tt("dve", t0, li1, a_im, ALU.mult)
            tt("dve", gr, gr, t0, ALU.add)
            tt("dve", gr, gr, den, ALU.mult)
            tt("dve", gi, li1, a_re, ALU.mult)
            tt("dve", t0, nr, a_im, ALU.mult)
            tt("dve", gi, gi, t0, ALU.subtract)
            tt("dve", gi, gi, den, ALU.mult)
            bbr = V(o, [128, 16, 16]); o += 1024
            bbi = V(o, [128, 16, 16]); o += 1024
            tb_ = V(o, [128, 16, 16]); o += 1024
            Br = sbp[:, :, 0, :]
            Bi = sbp[:, :, 1, :]
            grb = gr.unsqueeze(2).broadcast_to([128, 16, 16])
            gib = gi.unsqueeze(2).broadcast_to([128, 16, 16])
            tt("dve", bbr, Br, grb, ALU.mult)
            tt("dve", tb_, Bi, gib, ALU.mult)
            tt("dve", bbr, bbr, tb_, ALU.subtract)
            tt("dve", bbi, Bi, grb, ALU.mult)
            tt("dve", tb_, Br, gib, ALU.mult)
            tt("dve", bbi, bbi, tb_, ALU.add)
            Cr = scp[:, :, 0, :]
            Ci = scp[:, :, 1, :]
            big = [V(o + i * 2048, [128, 4, 8, 16]) for i in range(9)]; o += 9 * 2048
            assert o <= RING0, o
            Pr, Pi, WSr, WSi, Qr, Qi, WXr, WXi, tmpb = big
            MFb = consts[:, C_MF:C_MF + 128].unsqueeze(1).broadcast_to([128, 4, 128])
            MBb = consts[:, C_MB:C_MB + 128].unsqueeze(1).broadcast_to([128, 4, 128])

            def cmul(outr, outi, slot0, vr, vi, gs, neg_i=False):
                lr = LR[:, gs, slot0:slot0 + 8].unsqueeze(3).broadcast_to([128, 4, 8, 16])
                li = LI[:, gs, slot0:slot0 + 8].unsqueeze(3).broadcast_to([128, 4, 8, 16])
                vrb = vr[:, gs, :].unsqueeze(2).broadcast_to([128, 4, 8, 16])
                vib = vi[:, gs, :].unsqueeze(2).broadcast_to([128, 4, 8, 16])
                tt("dve", outr, lr, vrb, ALU.mult)
                tt("pool", tmpb, li, vib, ALU.mult)
                tt("dve", outr, outr, tmpb, ALU.subtract)
                if not neg_i:
                    tt("dve", outi, lr, vib, ALU.mult)
                    tt("pool", tmpb, li, vrb, ALU.mult)
                    tt("dve", outi, outi, tmpb, ALU.add)
                else:
                    tt("dve", outi, lr, vib, ALU.mult)
                    tt("pool", tmpb, li, vrb, ALU.mult)
                    tt("dve", outi, outi, tmpb, ALU.add)
                    ts("dve", outi, outi, -1.0, None, ALU.mult)

            if STOP[0] == 20:
                return
            for qq in range(4):
                gs = slice(4 * qq, 4 * qq + 4)
                g0 = 4 * qq
                cmul(Pr, Pi, 0, bbr, bbi, gs)
                cmul(WSr, WSi, 8, bbr, bbi, gs)
                cmul(Qr, Qi, 16, Cr, Ci, gs, neg_i=True)
                cmul(WXr, WXi, 24, Cr, Ci, gs, neg_i=True)
                if STOP[0] == 21:
                    return
                f2 = lambda a_: a_.rearrange("p g s j -> p g (s j)")
                Pr2, Pi2, WSr2, WSi2, Qr2, Qi2, WXr2, WXi2 = [f2(a_) for a_ in big[:8]]
                bF, bB = ps[0 + (qq % 2) * 2], ps[1 + (qq % 2) * 2]
                for gi_ in range(4):
                    cs_ = slice(gi_ * 128, (gi_ + 1) * 128)
                    mm(bF[:, cs_], Pr2[0:64, gi_, :], Qr2[0:64, gi_, :], start=True, stop=False, sig=False)
                    mm(bF[:, cs_], Pi2[0:64, gi_, :], Qi2[0:64, gi_, :], start=False, stop=True, sig=(gi_ == 3))
                for gi_ in range(4):
                    cs_ = slice(gi_ * 128, (gi_ + 1) * 128)
                    mm(bB[:, cs_], Pr2[64:128, gi_, :], Qr2[64:128, gi_, :], start=True, stop=False, sig=False)
                    mm(bB[:, cs_], Pi2[64:128, gi_, :], Qi2[64:128, gi_, :], start=False, stop=True, sig=(gi_ == 3))
                tmpM = tmpb.rearrange("p g s j -> p g (s j)")
                tt("dve", tmpM, bF.rearrange("p (g m) -> p g m", g=4), MFb, ALU.mult)
                tt("dve", Pr2, bB.rearrange("p (g m) -> p g m", g=4), MBb, ALU.mult)
                tt("dve", tmpM, tmpM, Pr2, ALU.add)
                for gi_ in range(4):
                    stt("dve", M_sb[:, g0 + gi_, :], ident_f, sdp[:, g0 + gi_:g0 + gi_ + 1], tmpM[:, gi_, :],
                        ALU.mult, ALU.add)
                if STOP[0] == 22:
                    return
                for ri, arr in enumerate((WSr2, WSi2)):
                    bank = ps[4 + ri]
                    for gi_ in range(4):
                        tr(bank[:, gi_ * 128:(gi_ + 1) * 128], arr[:, gi_, :], ident_f)
                    cp("act", WS_sb[:, g0:g0 + 4, ri, :], bank.rearrange("p (g m) -> p g m", g=4))
                cp("act", WX_sb[:, gs, 0, :], WXr2)
                cp("act", WX_sb[:, gs, 1, :], WXi2)

            if STOP[0] == 23:
                return
            o = o_run
            U = V(o, [128, 16, 256], BF16); o += 8192
            Y8 = V(O_YT + 6 * 4096, [128, 2, 8, 256], BF16)
            Z8g = V(o, [128, 2, 16, 8, 16], BF16)
            NB = 4
            arrs = [V(o + i * NB * 1024, [128, NB, 256]) for i in range(6)]; o += 6 * NB * 1024
            cs_t, sn_t, Wr, Wi, Wr2, Wi2 = arrs
            Xr = V(o, [128, NB, 258], BF16); o += NB * 516
            Xi = V(o, [128, NB, 258], BF16); o += NB * 516
            Yg = V(o, [128, 256], BF16); o += 512
            g1 = V(o, [128, 256]); o += 1024
            assert o <= RING0, o
            wss = wCD[:, :, 512:768]
            ptr_bf = ps[7].bitcast(BF16)
            for cb in range(2):
                for s in range(8):
                    bank = ps[4 + (cb * 8 + s) % 3]
                    c0 = 1024 * cb + s
                    for k in range(8):
                        mm(bank[:, 0:256], hT[:, k, c0:c0 + 1017:8], wss[:, k, :], start=(k == 0), stop=(k == 7))
                    cp("act", Z8g[:, cb, :, s, :], bank[:, 0:256].rearrange("p (g j) -> p g j", g=16))
            for g4 in range(4):
                for gi_ in range(4):
                    g = g4 * 4 + gi_
                    for cb in range(2):
                        tr(ptr_bf[:, gi_ * 256 + cb * 128: gi_ * 256 + (cb + 1) * 128],
                           Z8g[:, cb, g, :, :].rearrange("p s j -> p (s j)"), ident_bf)
                cp("dve", U[:, g4 * 4:(g4 + 1) * 4, :], ptr_bf.rearrange("p (g c) -> p g c", g=4))
            if STOP[0] == 24:
                return
            memset("dve", Xr[:, :, 0:1], 0.0)
            memset("dve", Xi[:, :, 0:1], 0.0)
            memset("dve", Xr[:, :, 255:257], 0.0)
            memset("dve", Xi[:, :, 255:257], 0.0)
            cidx = consts[:, C_CIDX:C_CIDX + 256].unsqueeze(1).broadcast_to([128, NB, 256])
            for gb in range(16 // NB):
                gsl = slice(gb * NB, gb * NB + NB)
                for gi_ in range(NB):
                    g = gb * NB + gi_
                    mm(psA[:, gi_ * 256:(gi_ + 1) * 256], WS_sb[:, g, 0, :], U[:, g, :], sig=(gi_ % 2 == 1))
                for gi_ in range(NB):
                    g = gb * NB + gi_
                    mm(psB[:, gi_ * 256:(gi_ + 1) * 256], WS_sb[:, g, 1, :], U[:, g, :], sig=(gi_ % 2 == 1))
                tt("dve", Wr2, cidx, th8[:, gsl].unsqueeze(2).broadcast_to([128, NB, 256]), ALU.mult)
                sincos(Wr2, sn_t, cs_t, Wr, Wi)
                rot = []
                for d_ in range(2):
                    pp = slice(64 * d_, 64 * d_ + 64)
                    sr = psA[pp, :].rearrange("p (g c) -> p g c", g=NB)
                    si = psB[pp, :].rearrange("p (g c) -> p g c", g=NB)
                    if d_ == 1:
                        sr = sr[:, :, ::-1]
                        si = si[:, :, ::-1]
                    rot.append((pp, sr, si, cs_t[pp], sn_t[pp], Wr2[pp], Wi2[pp]))
                for (pp, sr, si, c_, s_, a_, b_) in rot:
                    tt("dve", a_, sr, c_, ALU.mult)
                    tt("dve", b_, si, s_, ALU.mult)
                for (pp, sr, si, c_, s_, a_, b_) in rot:
                    tt("pool", Wr[pp], a_, b_, ALU.add)
                for (pp, sr, si, c_, s_, a_, b_) in rot:
                    tt("dve", a_, si, c_, ALU.mult)
                    tt("dve", b_, sr, s_, ALU.mult)
                for (pp, sr, si, c_, s_, a_, b_) in rot:
                    tt("pool", Wi[pp], a_, b_, ALU.subtract)
                if STOP[0] == 25:
                    return
                for gi_ in range(NB):
                    g = gb * NB + gi_
                    r8b = r8[:, g:g + 1].broadcast_to([128, 256])
                    for W_, W2_ in ((Wr, Wr2), (Wi, Wi2)):
                        S.op("dve", lambda W_=W_, W2_=W2_, gi_=gi_, r8b=r8b: nc.vector.tensor_tensor_scan(
                            W2_[:, gi_, :], r8b, W_[:, gi_, :], 0.0, ALU.mult, ALU.add),
                            reads=[r8b, W_[:, gi_, :]], writes=[W2_[:, gi_, :]])
                if STOP[0] == 26:
                    return
                unr = []
                for d_ in range(2):
                    pp = slice(64 * d_, 64 * d_ + 64)
                    if d_ == 0:
                        oxr, oxi = Xr[pp, :, 1:257], Xi[pp, :, 1:257]
                        rd_ = lambda t_: t_
                    else:
                        oxr, oxi = Xr[pp, :, 0:255], Xi[pp, :, 0:255]
                        rd_ = lambda t_: t_[:, :, 254::-1]
                    unr.append((pp, Wr[pp], Wi[pp], oxr, oxi, rd_))
                for (pp, a_, b_, oxr, oxi, rd_) in unr:
                    tt("dve", a_, Wr2[pp], cs_t[pp], ALU.mult)
                    tt("pool", b_, Wi2[pp], sn_t[pp], ALU.mult)
                for (pp, a_, b_, oxr, oxi, rd_) in unr:
                    tt("dve", oxr, rd_(a_), rd_(b_), ALU.subtract)
                for (pp, a_, b_, oxr, oxi, rd_) in unr:
                    tt("dve", a_, Wr2[pp], sn_t[pp], ALU.mult)
                    tt("pool", b_, Wi2[pp], cs_t[pp], ALU.mult)
                for (pp, a_, b_, oxr, oxi, rd_) in unr:
                    tt("dve", oxi, rd_(a_), rd_(b_), ALU.add)
                if STOP[0] == 31:
                    return
                for gi_ in range(NB):
                    g = gb * NB + gi_
                    bank = ps[4 + gi_ % 2][:, 0:256]
                    mm(bank, M_sb[:, g, :], U[:, g, :], start=True, stop=False, sig=False)
                    mm(bank, WX_sb[:, g, 0, :], Xr[:, gi_, 0:256], start=False, stop=False, sig=False)
                    mm(bank, WX_sb[:, g, 1, :], Xi[:, gi_, 0:256], start=False, stop=True)
                    if STOP[0] == 32:
                        return
                    act(g1, bank, AF.Square)
                    ts("dve", g1, g1, 0.044715, 1.0, ALU.mult, ALU.add)
                    tt("dve", g1, g1, bank, ALU.mult)
                    act(g1, g1, AF.Sigmoid, scale=1.5957691216057308)
                    tt("dve", Yg, g1, bank, ALU.mult)
                    if STOP[0] == 33:
                        return
                    for cb in range(2):
                        tr(ptr_bf[:, 512 + cb * 128:512 + (cb + 1) * 128], Yg[:, cb * 128:(cb + 1) * 128], ident_bf)
                    if STOP[0] == 34:
                        return
                    for cb in range(2):
                        cp("dve", Y8[:, cb, :, 16 * g:16 * g + 16],
                           ptr_bf[:, 512 + cb * 128:512 + (cb + 1) * 128].rearrange("p (s i) -> p s i", s=8))
            if STOP[0] == 27:
                return
            ysT = V(o_run, [128, 2, L], BF16)
            for cb in range(2):
                for m in range(2):
                    for s in range(8):
                        tr(ptr_bf[:, s * 128:(s + 1) * 128], Y8[:, cb, s, m * 128:(m + 1) * 128], ident_bf)
                    cp("dve", ysT[:, m, 1024 * cb:1024 * (cb + 1)].rearrange("p (c s) -> p s c", s=8),
                       ptr_bf.rearrange("p (s c) -> p s c", s=8))
            if STOP[0] == 28:
                return
            wgl = V(O_SCR, [128, 2, 256], BF16)
            sgb = [V(O_SCR + 1024 + i * 2048, [128, 512]) for i in range(2)]
            tmp = O_SCR + 5120
            assert tmp + 8192 <= o_run
            yss = V(o_run + 16384, [128, 2, L])
            assert o_run + 32768 <= SCR_END
            S.dma("pool", wgl, wglu_d[l].rearrange("(k p) n -> p k n", p=128))
            for m in range(2):
                for tb in range(4):
                    sl = slice(tb * 512, (tb + 1) * 512)
                    bank = ps[(m * 4 + tb) % 4]
                    for k in range(2):
                        mm(bank, wgl[:, k, m * 128:(m + 1) * 128], ysT[:, k, sl], start=(k == 0), stop=(k == 1))
                    sg = sgb[tb % 2]
                    act(sg, bank, AF.Sigmoid, bias=pvs[:, l, PV_BGLU + m:PV_BGLU + m + 1])
                    tt("dve", yss[:, m, sl], ysT[:, m, sl], sg, ALU.mult)
            gnorm_fm(yss, 3, l, tmp)


        def gu_unit(wg, wu, nf, aT, sgb):
            bi = 0
            for f in range(nf):
                for tb in range(4):
                    sl = slice(tb * 512, (tb + 1) * 512)
                    bg = ps[bi % 2]
                    bu = ps[2 + bi % 2]
                    bi += 1
                    for k in range(8):
                        mm(bg, wg[:, k, f * 128:(f + 1) * 128], hT[:, k, sl], start=(k == 0), stop=(k == 7))
                    sg = sgb[bi % 2]
                    act(sg, bg, AF.Silu)
                    for k in range(8):
                        mm(bu, wu[:, k, f * 128:(f + 1) * 128], hT[:, k, sl], start=(k == 0), stop=(k == 7))
                    tt("dve", aT[:, f, sl], bu, sg, ALU.mult)

        def ffn_dense(l):
            norm_fm(PV_N2, l, O_SCR + 49152)
            aTs = [V(O_YT + i * 16384, [128, 4, L], BF16) for i in range(2)]
            rg = WRing(O_SCR, 2, [128, 8, 512])
            ru = WRing(O_SCR + 16384, 2, [128, 8, 512])
            rd = WRing(O_SCR + 32768, 2, [128, 4, 1024])
            sgb = [V(O_SCR + 49152 + i * 1024, [128, 512], BF16) for i in range(2)]
            assert O_SCR + 49152 + 8192 <= RING0
            fgv = fg_d.rearrange("(k p) n -> p k n", p=128)
            fuv = fu_d.rearrange("(k p) n -> p k n", p=128)
            fdv = fd_d.rearrange("(f p) n -> p f n", p=128)
            units = [(i * 4, 4) for i in range(5)] + [(20, 2)]
            nu = len(units)
            bufs = {}

            def ld_gu(u):
                f0, nf = units[u]
                wg = rg.load(fgv[:, :, f0 * 128:(f0 + nf) * 128], sub=lambda b: b[:, :, 0:nf * 128])
                wu = ru.load(fuv[:, :, f0 * 128:(f0 + nf) * 128], sub=lambda b: b[:, :, 0:nf * 128])
                bufs[("gu", u)] = (wg, wu)

            def ld_d(u):
                f0, nf = units[u]
                bufs[("d", u)] = rd.load(fdv[:, f0:f0 + nf, :], sub=lambda b: b[:, 0:nf, :])

            def down(u):
                f0, nf = units[u]
                wd = bufs.pop(("d", u))
                aT = aTs[u % 2]
                bi = 0
                for m in range(8):
                    for tb in range(4):
                        sl = slice(tb * 512, (tb + 1) * 512)
                        bank = ps[4 + bi % 3]
                        bi += 1
                        for f in range(nf):
                            mm(bank, wd[:, f, m * 128:(m + 1) * 128], aT[:, f, sl], start=(f == 0), stop=(f == nf - 1))
                        tt("dve" if bi % 2 == 0 else "dve", xT[:, m, sl], xT[:, m, sl], bank, ALU.add)

            ld_gu(0); ld_gu(1); ld_d(0); ld_d(1)
            for u in range(nu):
                wg, wu = bufs.pop(("gu", u))
                gu_unit(wg, wu, units[u][1], aTs[u % 2], sgb)
                if u + 2 < nu:
                    ld_gu(u + 2)
                if u >= 1:
                    down(u - 1)
                    if u + 1 < nu:
                        ld_d(u + 1)
            down(nu - 1)

        def moe_and_final(l):
            norm_fm(PV_N2, l, O_SCR + 49152)
            O_XTOK = O_YT
            x_tok = V(O_XTOK, [128, 16, D])
            o = O_XTOK + 65536
            rd = WRing(o, 2, [128, 4, 1024]); o += 16384
            sgb = [V(o + i * 1024, [128, 512], BF16) for i in range(2)]; o += 2048
            cw = V(o, [128, 16, 8]); o += 512
            lg = V(o, [128, 16, 8]); o += 512
            lg2 = V(o, [128, 16, 8]); o += 512
            eq1 = V(o, [128, 16, 8]); o += 512
            eq2 = V(o, [128, 16, 8]); o += 512
            wr_sb = V(o, [128, 8, 8]); o += 256
            wrg = V(o, [128, 8, 8]); o += 256
            sm_ = [V(o + i * 64, [128, 16]) for i in range(8)]; o += 512
            ssq, rstd_t, m1, m2, dd, ee, g1_, g2_ = sm_
            gfin = V(o, [128, D]); o += 4096
            junk = V(o, [128, D]); o += 4096
            assert o <= SCR_END, o
            S.dma("sp", wr_sb, wr_d)
            S.dma("sp", gfin, gfin_d)
            for t in range(16):
                for hf in range(2):
                    bank = ps[(t * 2 + hf) % 4]
                    for kk in range(4):
                        k = hf * 4 + kk
                        tr(bank[:, kk * 128:(kk + 1) * 128], xT[:, k, t * 128:(t + 1) * 128], ident_f)
                    cp("act" if hf == 0 else "dve", x_tok[:, t, hf * 512:(hf + 1) * 512], bank)
            for t in range(16):
                act(junk, x_tok[:, t, :], AF.Square, accum=ssq[:, t:t + 1])
            act(rstd_t, ssq, AF.Sqrt, bias=eps_col, scale=1.0 / D)
            recip(rstd_t, rstd_t)
            for k in range(8):
                ts("dve", wrg[:, k, :], wr_sb[:, k, :], pvs[:, l, PV_N2 + k:PV_N2 + k + 1], None, ALU.mult)
            lb = ps[7]
            for t in range(16):
                for k in range(8):
                    mm(lb[:, t * 8:(t + 1) * 8], xT[:, k, t * 128:(t + 1) * 128], wrg[:, k, :],
                       start=(k == 0), stop=(k == 7), sig=(k == 7 and t == 15), skip_group_check=True)
            b3 = lambda a_: a_.unsqueeze(2).broadcast_to([128, 16, 8])
            tt("dve", lg, lb[:, 0:128].rearrange("p (t e) -> p t e", e=8), b3(rstd_t), ALU.mult)
            S.op("dve", lambda: nc.vector.tensor_reduce(m1, lg, AX.X, ALU.max), reads=[lg], writes=[m1])
            tt("dve", eq1, lg, b3(m1), ALU.is_equal)
            stt("dve", lg2, eq1, -1e30, lg, ALU.mult, ALU.add)
            S.op("dve", lambda: nc.vector.tensor_reduce(m2, lg2, AX.X, ALU.max), reads=[lg2], writes=[m2])
            tt("dve", eq2, lg2, b3(m2), ALU.is_equal)
            tt("dve", dd, m2, m1, ALU.subtract)
            act(ee, dd, AF.Exp)
            ts("dve", g1_, ee, 1.0, None, ALU.add)
            recip(g1_, g1_)
            tt("dve", g2_, ee, g1_, ALU.mult)
            tt("dve", eq1, eq1, b3(g1_), ALU.mult)
            tt("dve", eq2, eq2, b3(g2_), ALU.mult)
            tt("dve", cw, eq1, eq2, ALU.add)
            aTs = [V(O_XT + i * 16384, [128, 4, L], BF16) for i in range(2)]
            rg = WRing(O_XT + 32768, 2, [128, 8, 512])
            ru = WRing(O_XT + 49152, 2, [128, 8, 512])
            NU = 7
            units = [(e, u) for e in range(NE) for u in range(NU)]
            bufs = {}

            def ld_gu(n):
                e, u = units[n]
                wg = rg.load(mg_d[e].rearrange("(k p) n -> p k n", p=128)[:, :, u * 512:(u + 1) * 512])
                wu = ru.load(mu_d[e].rearrange("(k p) n -> p k n", p=128)[:, :, u * 512:(u + 1) * 512])
                bufs[("gu", n)] = (wg, wu)

            def ld_d(n):
                e, u = units[n]
                bufs[("d", n)] = rd.load(md_d[e].rearrange("(f p) n -> p f n", p=128)[:, u * 4:(u + 1) * 4, :])

            def down(n):
                e, u = units[n]
                wd = bufs.pop(("d", n))
                aT = aTs[n % 2]
                bi = 0
                for t in range(16):
                    for hf in range(2):
                        bank = ps[4 + bi % 3]
                        bi += 1
                        for f in range(4):
                            mm(bank, aT[:, f, t * 128:(t + 1) * 128], wd[:, f, hf * 512:(hf + 1) * 512],
                               start=(f == 0), stop=(f == 3))
                        xs = x_tok[:, t, hf * 512:(hf + 1) * 512]
                        stt("dve", xs, bank, cw[:, t, e:e + 1], xs, ALU.mult, ALU.add)

            nun = len(units)
            ld_gu(0); ld_gu(1); ld_d(0); ld_d(1)
            for n in range(nun):
                wg, wu = bufs.pop(("gu", n))
                gu_unit(wg, wu, 4, aTs[n % 2], sgb)
                if n + 2 < nun:
                    ld_gu(n + 2)
                if n >= 1:
                    down(n - 1)
                    if n + 1 < nun:
                        ld_d(n + 1)
            down(nun - 1)
            for t in range(16):
                act(junk, x_tok[:, t, :], AF.Square, accum=ssq[:, t:t + 1])
            act(rstd_t, ssq, AF.Sqrt, bias=eps_col, scale=1.0 / D)
            recip(rstd_t, rstd_t)
            for t in range(16):
                stt("dve", x_tok[:, t, :], x_tok[:, t, :], rstd_t[:, t:t + 1], gfin, ALU.mult, ALU.mult)
                S.dma("sp", out_d[t * 128:(t + 1) * 128, :], x_tok[:, t, :])

        stop_after = STOP[0]
        winvs = [win_d[l].rearrange("(k p) n -> p k n", p=128) for l in range(2)]
        wA = ring_load(0, winvs[0][:, :, 0:768])
        for l in range(nlayers):
            winv = winvs[l]
            woutv = wout_d[l].rearrange("(k p) n -> p k n", p=128)
            with nc.named_scope(f"l{l}_norm1"):
                norm_fm(PV_N1, l, O_SCR + 16384)
            if dbg:
                dump(hT, 8)
            if stop_after == 0:
                break
            with nc.named_scope(f"l{l}_attn"):
                wB = ring_load(1, winv[:, :, 768:1536])
                mixer_attention(l, wA)
            if stop_after in (1, 10, 11, 12, 13, 14):
                dump(yT, 8); break
            with nc.named_scope(f"l{l}_sconv"):
                wCD = ring_load(0, winv[:, :, 1536:2304])
                mixer_sconv(l, wB)
            if stop_after == 2:
                dump(yT, 8); break
            with nc.named_scope(f"l{l}_conf"):
                mixer_conformer(l, wCD)
            if stop_after == 3:
                dump(yT, 8); break
            with nc.named_scope(f"l{l}_s5"):
                mixer_s5(l, wCD)
            if stop_after == 4 or 20 <= stop_after < 40:
                dump(yT, 8); break
            if dbg:
                dump(yT, 8)

            def add_res(m, tb, bank):
                sl = slice(tb * 512, (tb + 1) * 512)
                tt("dve", xT[:, m, sl], xT[:, m, sl], bank, ALU.add)
            with nc.named_scope(f"l{l}_wout"):
                w0 = ring_load(1, woutv[:, :, 0:512])
                w1 = ring_load(0, woutv[:, :, 512:1024])
                proj_w(w0, 0, 512, yT, 8, add_res, ps[0:4])
                proj_w(w1, 0, 512, yT, 8, lambda m, tb, bank: add_res(m + 4, tb, bank), ps[0:4])
            if dbg:
                dump(xT, 8)
            if l % 2 == 0:
                with nc.named_scope(f"l{l}_ffn"):
                    if l + 1 < nlayers:
                        wA = ring_load(0, winvs[l + 1][:, :, 0:768])
                    ffn_dense(l)
                if dbg:
                    dump(xT, 8)
                if nlayers == 1:
                    for k in range(8):
                        S.dma("sp", out_d.rearrange("(k p) d -> p k d", p=128)[:, k, :], xT[:, k, 0:1024])
            else:
                with nc.named_scope(f"l{l}_moe"):
                    moe_and_final(l)
        S.wait_all_dma("sp")
        print(f"[build] inst={S.n_inst} waits={S.n_wait} sems={S.nsem}", flush=True)
    return nc


_CACHE = {}


def kernel(**inputs):
    sh, per_core = host_prep(inputs)
    if "nc" not in _CACHE:
        _CACHE["nc"] = build_program(2, False)
    nc = _CACHE["nc"]
    in_maps = []
    for c in range(NCORES):
        m = dict(sh)
        m["xT"] = per_core[c]
        in_maps.append(m)
    res = run_bass_kernel_spmd(nc, in_maps, core_ids=list(range(NCORES)))
    out = np.stack([np.asarray(res.results[c]["out"], np.float32) for c in range(NCORES)], 0)
    return out
```
